# Optimizing a Trainium2 kernel written in Bass

```python
import jax, jax.numpy as jnp
from jax import lax
import numpy as np


D_MODEL = 1024
BATCH = 4
SEQ = 4096
DEPTH = 1

GRID_W = 64
CTX_LEN = 256

RET_HEADS = 8
RET_QK_DIM = 128
RET_V_DIM = 256
RET_QK = RET_HEADS * RET_QK_DIM
RET_V = RET_HEADS * RET_V_DIM
RET_CHUNK = 128
ROPE_BASE = 10000.0

CONV_DIM = D_MODEL
CONV_WIDTH = 31
N_BRANCHES = 2

N_GROUPS = 4
EXPERTS_PER_GROUP = 8
N_EXPERTS = N_GROUPS * EXPERTS_PER_GROUP
EXPERT_TOP_K = 2
EXPERT_FF = 512

LN_EPS = 1e-5
ALPHA = (2.0 * DEPTH) ** 0.25
BETA = (8.0 * DEPTH) ** -0.25

Q_OFF = 0
K_OFF = Q_OFF + RET_QK
V_OFF = K_OFF + RET_QK
G_OFF = V_OFF + RET_V
GLU_OFF = G_OFF + RET_V
GATE_OFF = GLU_OFF + 2 * CONV_DIM
IN_COLS = GATE_OFF + N_BRANCHES * D_MODEL

kernel_name = 'hybrid_retention_conformer_hmoe_block'


def _layernorm(x, g, b):
    xf = x.astype(jnp.float32)
    mu = xf.mean(-1, keepdims=True)
    var = jnp.square(xf - mu).mean(-1, keepdims=True)
    return (xf - mu) * lax.rsqrt(var + LN_EPS) * g + b


def _head_norm(o):
    of = o.astype(jnp.float32)
    mu = of.mean(-1, keepdims=True)
    var = jnp.square(of - mu).mean(-1, keepdims=True)
    return (of - mu) * lax.rsqrt(var + LN_EPS)


def _modulate(h, shift, scale):
    return h * (1.0 + scale) + shift


def _rope_2d(rows):
    r, col = jnp.meshgrid(jnp.arange(rows), jnp.arange(GRID_W), indexing='ij')
    r = r.reshape(-1).astype(jnp.float32)
    col = col.reshape(-1).astype(jnp.float32)
    n_freq = RET_QK_DIM // 4
    inv = ROPE_BASE ** (-jnp.arange(n_freq, dtype=jnp.float32) / n_freq)
    ang = jnp.concatenate([r[:, None] * inv, col[:, None] * inv], axis=-1)
    ang = jnp.concatenate([ang, ang], axis=-1)
    return jnp.cos(ang), jnp.sin(ang)


def _apply_rope(t, cos, sin):
    t1, t2 = jnp.split(t, 2, axis=-1)
    rot = jnp.concatenate([-t2, t1], axis=-1)
    return t * cos[None, :, None, :] + rot * sin[None, :, None, :]


def _split_proj(proj):
    b_, l_ = proj.shape[:2]
    q = proj[..., Q_OFF:K_OFF].reshape(b_, l_, RET_HEADS, RET_QK_DIM)
    k = proj[..., K_OFF:V_OFF].reshape(b_, l_, RET_HEADS, RET_QK_DIM) * RET_QK_DIM ** -0.5
    v = proj[..., V_OFF:G_OFF].reshape(b_, l_, RET_HEADS, RET_V_DIM)
    g = proj[..., G_OFF:GLU_OFF]
    glu = proj[..., GLU_OFF:GATE_OFF]
    gates = proj[..., GATE_OFF:]
    return q, k, v, g, glu, gates


def _retention_chunked(q, k, v, log_gamma, s0):
    b_, l_, h_, _ = q.shape
    dv = v.shape[-1]
    n_chunks = l_ // RET_CHUNK
    pos = jnp.arange(RET_CHUNK, dtype=jnp.float32)
    diff = pos[:, None] - pos[None, :]
    intra = jnp.where(diff >= 0, jnp.exp(jnp.maximum(diff, 0.0)[None] * log_gamma[:, None, None]), 0.0)
    q_dec = jnp.exp((pos[:, None] + 1.0) * log_gamma[None, :])
    k_dec = jnp.exp((RET_CHUNK - 1.0 - pos)[:, None] * log_gamma[None, :])
    c_dec = jnp.exp(RET_CHUNK * log_gamma)

    def to_chunks(t):
        return jnp.moveaxis(t.reshape(b_, n_chunks, RET_CHUNK, h_, t.shape[-1]), 1, 0)

    def step(s, qkv):
        qc, kc, vc = qkv
        scores = jnp.einsum('bihd,bjhd->bhij', qc, kc) * intra
        o = (jnp.einsum('bhij,bjhv->bihv', scores, vc)
             + jnp.einsum('bihd,bhdv->bihv', qc * q_dec[None, :, :, None], s))
        s = s * c_dec[None, :, None, None] + jnp.einsum('bjhd,bjhv->bhdv', kc * k_dec[None, :, :, None], vc)
        return s, o

    s_fin, o = lax.scan(step, s0, (to_chunks(q), to_chunks(k), to_chunks(v)))
    o = jnp.moveaxis(o, 0, 1).reshape(b_, l_, h_, dv)
    return o, s_fin


def _retention_bidir(q, k, v, lg_f, lg_b, s0_f, s0_b):
    o_f, s_f = _retention_chunked(q, k, v, lg_f, s0_f)
    o_b, s_b = _retention_chunked(q[:, ::-1], k[:, ::-1], v[:, ::-1], lg_b, s0_b)
    return o_f + o_b[:, ::-1], s_f, s_b


def _retention_final_state(k, v, log_gamma):
    l_ = k.shape[1]
    w = jnp.exp((l_ - 1.0 - jnp.arange(l_, dtype=jnp.float32))[:, None] * log_gamma[None, :])
    return jnp.einsum('blhd,blhv->bhdv', k * w[None, :, :, None], v)


def _depthwise_conv(z, w, b):
    out = lax.conv_general_dilated(
        z, w[:, None, :].astype(z.dtype), window_strides=(1,),
        padding=[(CONV_WIDTH // 2, CONV_WIDTH // 2)],
        dimension_numbers=('NWC', 'WIO', 'NWC'), feature_group_count=z.shape[-1])
    return out + b


def _merge_branches(o_ret, g, glu, gates, p):
    b_, l_ = g.shape[:2]
    ret = jax.nn.silu(g.astype(jnp.float32)) * _head_norm(o_ret).reshape(b_, l_, RET_V)
    y_a = ret @ p['w_ret_out']
    za, zb = jnp.split(glu, 2, axis=-1)
    z = za * jax.nn.sigmoid(zb)
    z = _depthwise_conv(z, p['conv_dw'], p['conv_dw_b'])
    z = jax.nn.silu(_layernorm(z, p['conv_ln_g'], p['conv_ln_b']))
    y_b = z @ p['w_conv_out'] + p['b_conv_out']
    ga, gb = jnp.split(jax.nn.sigmoid(gates.astype(jnp.float32)), 2, axis=-1)
    return (ga * y_a + gb * y_b) @ p['w_mix_out'] + p['b_mix_out']


def _hier_moe(u, p):
    b_, l_, d_ = u.shape
    t = u.reshape(b_ * l_, d_)
    grp_p = jax.nn.softmax((t @ p['w_router_grp'] + p['b_router_grp']).astype(jnp.float32), axis=-1)
    grp_w, grp_idx = lax.top_k(grp_p, 1)
    exp_logits = (t @ p['w_router_exp'] + p['b_router_exp']).astype(jnp.float32)
    exp_logits = exp_logits.reshape(-1, N_GROUPS, EXPERTS_PER_GROUP)
    in_grp = jnp.take_along_axis(exp_logits, grp_idx[:, :, None], axis=1)[:, 0]
    top_w, top_i = lax.top_k(jax.nn.softmax(in_grp, axis=-1), EXPERT_TOP_K)
    top_w = top_w / top_w.sum(-1, keepdims=True) * grp_w
    eid = grp_idx * EXPERTS_PER_GROUP + top_i
    combine = jnp.einsum('nk,nke->ne', top_w, jax.nn.one_hot(eid, N_EXPERTS, dtype=jnp.float32))
    y = jnp.zeros(t.shape, jnp.float32)
    for e in range(N_EXPERTS):
        h = jax.nn.silu(t @ p['w_exp_gate'][e]) * (t @ p['w_exp_up'][e])
        y = y + combine[:, e:e + 1] * (h @ p['w_exp_down'][e])
    return y.reshape(b_, l_, d_)


def _layer(h_lat, h_ctx, s_lat, s_ctx, p, cos, sin, update_ctx):
    mod_lat = s_lat @ p['w_ada'] + p['b_ada']
    mod_ctx = s_ctx @ p['w_ada'] + p['b_ada']
    sh_m, sc_m, gt_m, sh_f, sc_f, gt_f = jnp.split(mod_lat[:, None, :], 6, axis=-1)
    csh_m, csc_m, cgt_m, csh_f, csc_f, cgt_f = jnp.split(mod_ctx, 6, axis=-1)
    lg_f = jax.nn.log_sigmoid(p['ret_decay_fwd'].astype(jnp.float32))
    lg_b = jax.nn.log_sigmoid(p['ret_decay_bwd'].astype(jnp.float32))
    b_ = h_lat.shape[0]

    u_ctx = _modulate(h_ctx, csh_m, csc_m)
    if update_ctx:
        q_c, k_c, v_c, g_c, glu_c, gates_c = _split_proj(u_ctx @ p['w_in'] + p['b_in'])
        zeros = jnp.zeros((b_, RET_HEADS, RET_QK_DIM, RET_V_DIM), jnp.float32)
        o_c, s_f, s_b = _retention_bidir(q_c, k_c, v_c, lg_f, lg_b, zeros, zeros)
    else:
        kv = u_ctx @ p['w_in'][:, K_OFF:G_OFF] + p['b_in'][K_OFF:G_OFF]
        k_c = kv[..., :RET_QK].reshape(b_, -1, RET_HEADS, RET_QK_DIM) * RET_QK_DIM ** -0.5
        v_c = kv[..., RET_QK:].reshape(b_, -1, RET_HEADS, RET_V_DIM)
        s_f = _retention_final_state(k_c, v_c, lg_f)
        s_b = _retention_final_state(k_c[:, ::-1], v_c[:, ::-1], lg_b)

    u_lat = _modulate(h_lat, sh_m, sc_m)
    q, k, v, g, glu, gates = _split_proj(u_lat @ p['w_in'] + p['b_in'])
    q = _apply_rope(q, cos, sin)
    k = _apply_rope(k, cos, sin)
    o, _, _ = _retention_bidir(q, k, v, lg_f, lg_b, s_f, s_b)
    mix = _merge_branches(o, g, glu, gates, p)
    h_lat = _layernorm(ALPHA * h_lat + gt_m * mix, p['ln1_g'], p['ln1_b'])
    moe = _hier_moe(_modulate(h_lat, sh_f, sc_f), p)
    h_lat = _layernorm(ALPHA * h_lat + gt_f * moe, p['ln2_g'], p['ln2_b'])

    if update_ctx:
        mix_c = _merge_branches(o_c, g_c, glu_c, gates_c, p)
        h_ctx = _layernorm(ALPHA * h_ctx + cgt_m * mix_c, p['ln1_g'], p['ln1_b'])
        moe_c = _hier_moe(_modulate(h_ctx, csh_f, csc_f), p)
        h_ctx = _layernorm(ALPHA * h_ctx + cgt_f * moe_c, p['ln2_g'], p['ln2_b'])
    return h_lat, h_ctx


def setup_inputs(seed: int = 0) -> dict:
    key = jax.random.key(seed)
    ks = jax.random.split(key, 32)
    f32 = jnp.float32
    D = D_MODEL

    def nrm(k, shape, scale):
        return jax.random.normal(k, shape, f32) * scale

    gamma0 = 1.0 - 2.0 ** (-5.0 - np.arange(RET_HEADS))
    logit0 = jnp.asarray(np.log(gamma0 / (1.0 - gamma0)), f32)
    return {
        'x': nrm(ks[0], (BATCH, SEQ, D), 1.0),
        'c': nrm(ks[1], (BATCH, D), 1.0),
        'ctx': nrm(ks[2], (BATCH, CTX_LEN, D), 1.0),
        'c_ctx': nrm(ks[3], (D,), 1.0),
        'w_ada': nrm(ks[4], (DEPTH, D, 6 * D), 0.5 * D ** -0.5),
        'b_ada': nrm(ks[5], (DEPTH, 6 * D), 0.01),
        'w_in': nrm(ks[6], (DEPTH, D, IN_COLS), D ** -0.5),
        'b_in': nrm(ks[7], (DEPTH, IN_COLS), 0.01),
        'ret_decay_fwd': logit0 + nrm(ks[8], (DEPTH, RET_HEADS), 0.1),
        'ret_decay_bwd': logit0 + nrm(ks[9], (DEPTH, RET_HEADS), 0.1),
        'w_ret_out': nrm(ks[10], (DEPTH, RET_V, D), RET_V ** -0.5),
        'conv_dw': nrm(ks[11], (DEPTH, CONV_WIDTH, CONV_DIM), CONV_WIDTH ** -0.5),
        'conv_dw_b': nrm(ks[12], (DEPTH, CONV_DIM), 0.01),
        'conv_ln_g': 1.0 + nrm(ks[13], (DEPTH, CONV_DIM), 0.02),
        'conv_ln_b': nrm(ks[14], (DEPTH, CONV_DIM), 0.01),
        'w_conv_out': nrm(ks[15], (DEPTH, CONV_DIM, D), CONV_DIM ** -0.5),
        'b_conv_out': nrm(ks[16], (DEPTH, D), 0.01),
        'w_mix_out': nrm(ks[17], (DEPTH, D, D), BETA * D ** -0.5),
        'b_mix_out': nrm(ks[18], (DEPTH, D), 0.01),
        'ln1_g': 1.0 + nrm(ks[19], (DEPTH, D), 0.02),
        'ln1_b': nrm(ks[20], (DEPTH, D), 0.01),
        'w_router_grp': nrm(ks[21], (DEPTH, D, N_GROUPS), D ** -0.5),
        'b_router_grp': nrm(ks[22], (DEPTH, N_GROUPS), 0.01),
        'w_router_exp': nrm(ks[23], (DEPTH, D, N_EXPERTS), D ** -0.5),
        'b_router_exp': nrm(ks[24], (DEPTH, N_EXPERTS), 0.01),
        'w_exp_gate': nrm(ks[25], (DEPTH, N_EXPERTS, D, EXPERT_FF), D ** -0.5),
        'w_exp_up': nrm(ks[26], (DEPTH, N_EXPERTS, D, EXPERT_FF), D ** -0.5),
        'w_exp_down': nrm(ks[27], (DEPTH, N_EXPERTS, EXPERT_FF, D), BETA * EXPERT_FF ** -0.5),
        'ln2_g': 1.0 + nrm(ks[28], (DEPTH, D), 0.02),
        'ln2_b': nrm(ks[29], (DEPTH, D), 0.01),
    }


def reference(x, c, ctx, c_ctx, w_ada, b_ada, w_in, b_in, ret_decay_fwd, ret_decay_bwd, w_ret_out,
              conv_dw, conv_dw_b, conv_ln_g, conv_ln_b, w_conv_out, b_conv_out, w_mix_out, b_mix_out,
              ln1_g, ln1_b, w_router_grp, b_router_grp, w_router_exp, b_router_exp,
              w_exp_gate, w_exp_up, w_exp_down, ln2_g, ln2_b):
    rows = x.shape[1] // GRID_W
    cos, sin = _rope_2d(rows)
    s_lat = jax.nn.silu(c)
    s_ctx = jax.nn.silu(c_ctx)
    h_lat, h_ctx = x, ctx
    for layer in range(DEPTH):
        p = dict(
            w_ada=w_ada[layer], b_ada=b_ada[layer], w_in=w_in[layer], b_in=b_in[layer],
            ret_decay_fwd=ret_decay_fwd[layer], ret_decay_bwd=ret_decay_bwd[layer],
            w_ret_out=w_ret_out[layer], conv_dw=conv_dw[layer], conv_dw_b=conv_dw_b[layer],
            conv_ln_g=conv_ln_g[layer], conv_ln_b=conv_ln_b[layer], w_conv_out=w_conv_out[layer],
            b_conv_out=b_conv_out[layer], w_mix_out=w_mix_out[layer], b_mix_out=b_mix_out[layer],
            ln1_g=ln1_g[layer], ln1_b=ln1_b[layer], w_router_grp=w_router_grp[layer],
            b_router_grp=b_router_grp[layer], w_router_exp=w_router_exp[layer],
            b_router_exp=b_router_exp[layer], w_exp_gate=w_exp_gate[layer], w_exp_up=w_exp_up[layer],
            w_exp_down=w_exp_down[layer], ln2_g=ln2_g[layer], ln2_b=ln2_b[layer])
        h_lat, h_ctx = _layer(h_lat, h_ctx, s_lat, s_ctx, p, cos, sin, layer + 1 < DEPTH)
    return h_lat
```

```python
import contextlib
import math
import os
import numpy as np
import ml_dtypes
import concourse.bass as bass
import concourse.mybir as mybir
from concourse.bass_utils import run_bass_kernel_spmd

F32 = mybir.dt.float32
BF16 = mybir.dt.bfloat16
AF = mybir.ActivationFunctionType
ALU = mybir.AluOpType

D = 1024
SEQ = 4096
NB = 4
T = 2048
NT = 16
NAUX = 2304
NTA = 18
H = 8
DK = 128
DV = 256
Q_OFF, K_OFF, V_OFF, G_OFF, GLU_OFF, GATE_OFF = 0, 1024, 2048, 4096, 6144, 8192
NE = 32
FF = 512
EPS = 1e-5
ALPHA = 2.0 ** 0.25
KSCALE = 128.0 ** -0.5
PI = math.pi

C_ID = 0
C_DF = 128
C_MF = 256
C_DB = 384
C_MB = 512
C_I1 = 640
C_I2 = 768
C_PC = 896
C_IE = 898
C_WE = 900
C_HM = 972
CW = 1004
CB_ID, CB_PR, CB_ON, CBW = 0, 128, 256, 384
R_BADA = 0
R_BIN = 48
R_CW = 128
R_CB = 376
R_LG = 384
R_LB = 392
R_BCO = 400
R_L1G = 408
R_L1B = 416
NVEC = 512


class Buf:
    __slots__ = ("name", "w", "r", "sem", "cnt", "excl")

    def __init__(self, name, excl=False):
        self.name = name
        self.excl = excl
        self.w = None
        self.r = []
        self.sem = None
        self.cnt = 0


class Sched:
    def __init__(self, nc, es):
        self.nc = nc
        self.es = es
        self.eng = {"pe": nc.tensor, "act": nc.scalar, "dve": nc.vector, "pool": nc.gpsimd, "sp": nc.sync}
        self.sem = {}
        self.cnt = {}
        for k in self.eng:
            self.sem[k] = es.enter_context(nc.semaphore("s_" + k))
            self.cnt[k] = 0
        self.seen = {k: {} for k in self.eng}
        self.dsem = {}
        self.nd = 0

    def _waits(self, e, evs):
        need = {}
        for ev in evs:
            if ev is None:
                continue
            k, v = ev
            if k == e and e == "pe":
                continue
            if v > need.get(k, 0):
                need[k] = v
        for k, v in need.items():
            if self.seen[e].get(k, 0) >= v:
                continue
            self.seen[e][k] = v
            s = self.sem[k] if k in self.sem else self.dsem[k]
            self.eng[e].wait_ge(s, v)

    def _deps(self, r, w):
        evs = []
        for b in r:
            evs.append(b.w)
            if b.excl:
                evs.extend(b.r)
        for b in w:
            evs.append(b.w)
            evs.extend(b.r)
        return evs

    def op(self, e, fn, r=(), w=()):
        self._waits(e, self._deps(r, w))
        ins = fn(self.eng[e])
        self.cnt[e] += 1
        ins.then_inc(self.sem[e], 1)
        ev = (e, self.cnt[e])
        for b in r:
            b.r.append(ev)
        for b in w:
            b.w = ev
            b.r = []
        return ins

    def dma(self, q, out, in_, owner, r=(), w=(), join=False):
        if owner.sem is None:
            self.nd += 1
            owner.sem = "d%d" % self.nd
            self.dsem[owner.sem] = self.es.enter_context(self.nc.semaphore("sd%d" % self.nd))
        evs = []
        if not join and owner.cnt > 0:
            evs.append((owner.sem, owner.cnt))
        for b in r:
            evs.append(b.w)
        for b in w:
            if not (join and b.w is not None and b.w[0] == owner.sem):
                evs.append(b.w)
            evs.extend(b.r)
        self._waits(q, evs)
        ins = self.eng[q].dma_start(out=out, in_=in_)
        owner.cnt += 16
        ins.then_inc(self.dsem[owner.sem], 16)
        ev = (owner.sem, owner.cnt)
        for b in r:
            b.r.append(ev)
        for b in w:
            b.w = ev
            b.r = []
        return ins

    def wait_all(self, e, bufs):
        evs = []
        for b in bufs:
            evs.append(b.w)
            evs.extend(b.r)
        self._waits(e, evs)


def build_program(dbg=None, stage=None):
    nc = bass.Bass("TRN2", target_bir_lowering=False)

    def din(name, shape, dt=F32):
        return nc.dram_tensor(name, list(shape), dt, kind="ExternalInput").ap()

    xo = din("xo", [T, D])
    xa = din("xa", [NAUX, D])
    xh = din("xh", [32, D])
    cvec = din("cvec", [D, 2])
    cst_d = din("cst", [128, CW])
    cstb_d = din("cstb", [128, CBW], BF16)
    vecs_d = din("vecs", [NVEC, 128])
    ango_d = din("ango", [128, T])
    anga_d = din("anga", [128, NAUX])
    w_ada = din("w_ada", [D, 6 * D])
    b_ada = din("b_ada", [1, 6 * D])
    w_in = din("w_in", [D, 10240])
    b_in = din("b_in", [1, 10240])
    decay = din("decay", [1, 16])
    w_ro = din("w_ret_out", [2048, D])
    w_co = din("w_conv_out", [D, D])
    w_mo = din("w_mix_out", [D, D])
    b_mo = din("b_mix_out", [1, D])
    ln1g = din("ln1_g", [1, D])
    ln1b = din("ln1_b", [1, D])
    ln2g = din("ln2_g", [1, D])
    ln2b = din("ln2_b", [1, D])
    w_rt = din("w_router", [D, 36])
    b_rt = din("b_router", [1, 36])
    w_eg = din("w_exp_gate", [NE, D, FF])
    w_eu = din("w_exp_up", [NE, D, FF])
    w_ed = din("w_exp_down", [NE, FF, D])
    out_d = nc.dram_tensor("out", [T, D], F32, kind="ExternalOutput").ap()
    if dbg is not None:
        dbg_d = nc.dram_tensor("dbg", list(dbg), F32, kind="ExternalOutput").ap()
    uaux_d = nc.dram_tensor("uaux_s", [128, 8, NAUX], BF16).ap()
    sina_d = nc.dram_tensor("sina_s", [128, NAUX], F32).ap()
    cosa_d = nc.dram_tensor("cosa_s", [128, NAUX], F32).ap()
    retT_d = nc.dram_tensor("retT_s", [128, 16, T], BF16).ap()
    h1_d = nc.dram_tensor("h1_s", [T, D], F32).ap()

    es = contextlib.ExitStack()
    with es:
        S = Sched(nc, es)

        all_bufs = []
        fence = []
        scoped = {}

        def sb(name, shape, dt=F32, stack=es):
            t = stack.enter_context(nc.sbuf_tensor("sb_" + name, list(shape), dt))
            b = Buf(name)
            all_bufs.append(b)
            b.r = list(fence)
            scoped.setdefault(id(stack), []).append(b)
            return t, b

        @contextlib.contextmanager
        def scope():
            st = contextlib.ExitStack()
            with st:
                yield st
                evs = {}
                for b in scoped.pop(id(st), []):
                    for ev in ([b.w] if b.w else []) + b.r:
                        if ev[1] > evs.get(ev[0], 0):
                            evs[ev[0]] = ev[1]
                for k, v in evs.items():
                    fence.append((k, v))

        def ps(name, shape, dt=F32, stack=es):
            t = stack.enter_context(nc.psum_tensor("pp_" + name, list(shape), dt))
            b = Buf(name, excl=True)
            all_bufs.append(b)
            return t, b

        cst, Bcst = sb("cst", [128, CW])
        cstb, Bcstb = sb("cstb", [128, CBW], BF16)
        fm, Bfm = sb("fm", [128, NVEC])
        modfm, Bmod = sb("modfm", [128, 48, 2])
        sc1m, Bsc1m = sb("sc1m", [128, 8, 2])
        modB, BmodB = sb("modB", [128, 24, 2])
        cv, Bcv = sb("cv", [128, 8, 2])
        sv, Bsv = sb("sv", [128, 8, 2], BF16)
        svr, Bsvr = sb("svr", [128, 8, 128], BF16)
        sc1f, Bsc1f = sb("sc1f", [128, 8])
        lg, Blg = sb("lg", [128, 16])
        cdec, Bcdec = sb("cdec", [128, 16])
        kdec, Bkdec = sb("kdec", [128, 16])
        waux, Bwaux = sb("waux", [128, NTA, 16])
        gtrow, Bgtrow = sb("gtrow", [128, 2, D])
        negpi, Bnegpi = sb("negpi", [128, 1])
        epsb, Bepsb = sb("epsb", [128, 1])
        combw, Bcombw = sb("combw", [128, NT, NE])
        dbuf = Buf("dram_misc")
        dbuf_ret = Buf("dram_ret")
        daux = [Buf("dram_aux%d" % i) for i in range(NTA + 20)]
        all_bufs.extend(daux)
        daux_n = [0]
        dbuf_h1 = Buf("dram_h1")
        all_bufs.extend([dbuf, dbuf_ret, dbuf_h1])

        S.dma("sp", cst[:], cst_d, Bcst, w=[Bcst])
        S.dma("sp", cstb[:], cstb_d, Bcstb, w=[Bcstb])
        S.op("dve", lambda e: e.memset(negpi[:], -PI), w=[Bnegpi])
        S.op("dve", lambda e: e.memset(epsb[:], EPS), w=[Bepsb])
        ident = cst[:, C_ID:C_ID + 128]
        identb = cstb[:, CB_ID:CB_ID + 128]
        protb = cstb[:, CB_PR:CB_PR + 128]
        onesb = cstb[:, CB_ON:CB_ON + 128]

        PS = [ps("ps%d" % i, [128, 512]) for i in range(6)]
        PSBs = [ps("psb%d" % i, [128, 1024], BF16) for i in range(2)]

        def dbg_dump(tile_ap, buf, rows, cols, col0=0):
            S.dma("sp", dbg_d[0:rows, col0:col0 + cols], tile_ap, buf, r=[buf])

        with scope() as p0:
            vt_, Bvt = sb("vt", [128, 4, 128], stack=p0)
            S.dma("sp", vt_[:], vecs_d.rearrange("(a p) c -> p a c", p=128), Bvt, w=[Bvt])
            pT, BpT = PS[0]
            for a in range(4):
                S.op("pe", lambda e, a=a: e.transpose(pT[:, a * 128:(a + 1) * 128], vt_[:, a, :], ident),
                     r=[Bvt, Bcst], w=[BpT])
            S.op("dve", lambda e: e.tensor_copy(fm[:], pT[:]), r=[BpT], w=[Bfm])

            S.dma("sp", cv[:], cvec.rearrange("(k p) c -> p k c", p=128), Bcv, w=[Bcv])
            S.op("act", lambda e: e.activation(sv[:], cv[:], AF.Silu), r=[Bcv], w=[Bsv])
            for k in range(8):
                S.op("dve", lambda e, k=k: e.tensor_copy(svr[:, k, :], sv[:, k, 0:1].to_broadcast([128, 128])),
                     r=[Bsv], w=[Bsvr])
            barow, Bbarow = sb("barow", [128, 2, D], stack=p0)
            for i, c0 in enumerate((2 * D, 5 * D)):
                S.dma("sp", barow[:, i, :], b_ada[:, c0:c0 + D].partition_broadcast(128), Bbarow, w=[Bbarow],
                      join=True)
            wad, Bwad = sb("wad", [128, 8, 1536], BF16, stack=p0)
            pM, BpM = PS[1]

            def ada_piece(piece, wad, Bwad, pM, BpM, jbase, pRs, barow, Bbarow):
                for k in range(8):
                    S.dma("pool", wad[:, k, :], w_ada[k * 128:(k + 1) * 128, piece * 1536:(piece + 1) * 1536],
                          Bwad, w=[Bwad], join=(k > 0))
                for j in range(12):
                    jj = piece * 12 + j - jbase
                    for k in range(8):
                        S.op("pe", lambda e, j=j, jj=jj, k=k: e.matmul(
                            pM[:, jj * 2:jj * 2 + 2], lhsT=wad[:, k, j * 128:(j + 1) * 128], rhs=sv[:, k, :],
                            start=(k == 0), stop=(k == 7)), r=[Bwad, Bsv], w=[BpM])
                for gi, c0 in enumerate((2 * D, 5 * D)):
                    for hf in range(2):
                        cc = c0 + hf * 512
                        if cc // 1536 != piece and (cc + 511) // 1536 != piece:
                            continue
                        lo = max(cc, piece * 1536)
                        hi = min(cc + 512, (piece + 1) * 1536)
                        pR, BpR = pRs[gi]
                        for k in range(8):
                            S.op("pe", lambda e, k=k, lo=lo, hi=hi, pR=pR: e.matmul(
                                pR[:, 0:hi - lo], lhsT=svr[:, k, :],
                                rhs=wad[:, k, lo - piece * 1536:hi - piece * 1536],
                                start=(k == 0), stop=(k == 7)), r=[Bwad, Bsvr], w=[BpR])
                        S.op("dve", lambda e, lo=lo, hi=hi, pR=pR, gi=gi, c0=c0: e.tensor_tensor(
                            gtrow[:, gi, lo - c0:hi - c0], pR[:, 0:hi - lo], barow[:, gi, lo - c0:hi - c0], ALU.add),
                            r=[BpR, Bbarow], w=[Bgtrow])

            for piece in range(2):
                ada_piece(piece, wad, Bwad, pM, BpM, 0, [PS[2], PS[3]], barow, Bbarow)
            S.op("dve", lambda e: e.tensor_tensor(
                modfm[:, 0:24, :], pM[:, 0:48].rearrange("p (j c) -> p j c", c=2),
                fm[:, R_BADA:R_BADA + 24].unsqueeze(2).to_broadcast([128, 24, 2]), ALU.add),
                r=[BpM, Bfm], w=[Bmod])
            S.op("dve", lambda e: e.tensor_scalar_add(sc1m[:], modfm[:, 8:16, :], 1.0), r=[Bmod], w=[Bsc1m])

            dc, Bdc = sb("dc", [128, 16], stack=p0)
            S.dma("sp", dc[:], decay.partition_broadcast(128), Bdc, w=[Bdc])
            S.op("act", lambda e: e.activation(dc[:], dc[:], AF.Exp, scale=-1.0), r=[Bdc], w=[Bdc])
            S.op("dve", lambda e: e.tensor_scalar_add(dc[:], dc[:], 1.0), r=[Bdc], w=[Bdc])
            S.op("act", lambda e: e.activation(lg[:], dc[:], AF.Ln), r=[Bdc], w=[Blg])
            S.op("dve", lambda e: e.tensor_scalar_mul(lg[:], lg[:], -1.0), r=[Blg], w=[Blg])
            S.op("act", lambda e: e.activation(cdec[:], lg[:], AF.Exp, scale=128.0), r=[Blg], w=[Bcdec])
            S.op("act", lambda e: e.activation(kdec[:, 0:8], lg[:, 0:8], AF.Exp, scale=cst[:, C_PC:C_PC + 1]),
                 r=[Blg, Bcst], w=[Bkdec])
            S.op("act", lambda e: e.activation(kdec[:, 8:16], lg[:, 8:16], AF.Exp, scale=cst[:, C_PC + 1:C_PC + 2]),
                 r=[Blg, Bcst], w=[Bkdec])
            S.op("dve", lambda e: e.tensor_scalar_mul(kdec[:], kdec[:], KSCALE), r=[Bkdec], w=[Bkdec])
            for t in range(NTA):
                for d_ in range(2):
                    c0 = C_WE + t * 4 + d_ * 2
                    S.op("act", lambda e, t=t, d_=d_, c0=c0: e.activation(
                        waux[:, t, d_ * 8:(d_ + 1) * 8], lg[:, d_ * 8:(d_ + 1) * 8], AF.Exp,
                        scale=cst[:, c0:c0 + 1]), r=[Blg, Bcst], w=[Bwaux])
                    S.op("dve", lambda e, t=t, d_=d_, c0=c0: e.tensor_scalar(
                        waux[:, t, d_ * 8:(d_ + 1) * 8], waux[:, t, d_ * 8:(d_ + 1) * 8],
                        cst[:, c0 + 1:c0 + 2], KSCALE, ALU.mult, ALU.mult), r=[Bwaux, Bcst], w=[Bwaux])

        if stage == "p0":
            dbg_dump(gtrow[:, 0, :], Bgtrow, 128, 1024, 0)
            dbg_dump(gtrow[:, 1, :], Bgtrow, 128, 1024, 1024)
            dbg_dump(modfm[:].rearrange("p a b -> p (a b)"), Bmod, 128, 96, 2048)
            dbg_dump(waux[:].rearrange("p a b -> p (a b)"), Bwaux, 128, 288, 2144)
            dbg_dump(kdec[:], Bkdec, 128, 16, 2432)
            dbg_dump(cdec[:], Bcdec, 128, 16, 2448)
            dbg_dump(lg[:], Blg, 128, 16, 2464)
            dbg_dump(fm[:], Bfm, 128, 512, 2480)
            S.wait_all("sp", [Bgtrow, Bmod, Bwaux, Bkdec, Bcdec, Blg, Bfm])
            return nc

        def mm(out, lhsT, rhs, start, stop, r, w):
            return S.op("pe", lambda e: e.matmul(out, lhsT=lhsT, rhs=rhs, start=start, stop=stop), r=r, w=w)

        u_own, Bu = sb("u_own", [128, 8, T], BF16)
        u_halo, Buh = sb("u_halo", [128, 8, 32], BF16)
        p12cm = scope()
        p12 = p12cm.__enter__()
        sino, Bsino = sb("sino", [128, T], stack=p12)
        coso, Bcoso = sb("coso", [128, T], stack=p12)
        inv, Binv = sb("inv", [128, 1], stack=p12)
        S.op("act", lambda e: e.activation(inv[:], cst[:, C_IE:C_IE + 1], AF.Exp, scale=math.log(10000.0)),
             r=[Bcst], w=[Binv])
        W = [dict(wq=sb("wq%d" % i, [128, 8, 128], BF16, stack=p12), wk=sb("wk%d" % i, [128, 8, 128], BF16, stack=p12),
                  wvg=sb("wvg%d" % i, [128, 8, 512], BF16, stack=p12), bvg=sb("bvgb%d" % i, [1, 512], BF16, stack=p12))
             for i in range(2)]

        def load_head_w(h):
            Wd = W[h % 2]
            wq, Bwq = Wd["wq"]; wk, Bwk = Wd["wk"]; wvg, Bwvg = Wd["wvg"]; bvg, Bbvg = Wd["bvg"]
            S.dma("pool", wq[:], w_in[:, Q_OFF + h * 128:Q_OFF + (h + 1) * 128].rearrange("(k p) c -> p k c", p=128),
                  Bwq, w=[Bwq])
            S.dma("pool", wk[:], w_in[:, K_OFF + h * 128:K_OFF + (h + 1) * 128].rearrange("(k p) c -> p k c", p=128),
                  Bwk, w=[Bwk])
            S.dma("pool", wvg[:, :, 0:256],
                  w_in[:, V_OFF + h * 256:V_OFF + (h + 1) * 256].rearrange("(k p) c -> p k c", p=128), Bwvg, w=[Bwvg])
            S.dma("pool", wvg[:, :, 256:512],
                  w_in[:, G_OFF + h * 256:G_OFF + (h + 1) * 256].rearrange("(k p) c -> p k c", p=128), Bwvg,
                  w=[Bwvg], join=True)
            S.dma("pool", bvg[:, 0:256], b_in[:, V_OFF + h * 256:V_OFF + (h + 1) * 256], Bbvg, w=[Bbvg])
            S.dma("pool", bvg[:, 256:512], b_in[:, G_OFF + h * 256:G_OFF + (h + 1) * 256], Bbvg, w=[Bbvg], join=True)

        load_head_w(0)
        with scope() as p1:
            xt = [sb("xt%d" % i, [128, D], stack=p1) for i in range(4)]
            ust = [sb("ust%d" % i, [128, 8, 128], BF16, stack=p1) for i in range(2)]
            utmp = [sb("utmp%d" % i, [128, 4, 128], stack=p1) for i in range(2)]
            ang, Bang = sb("ang", [128, 512], stack=p1)
            a1, Ba1 = sb("a1", [128, 512], stack=p1)
            a2, Ba2 = sb("a2", [128, 512], stack=p1)
            ai, Bai = sb("ai", [128, 512], mybir.dt.int32, stack=p1)
            so, Bso = sb("so", [128, 512], stack=p1)
            co, Bco = sb("co", [128, 512], stack=p1)
            inv2, Binv2 = sb("inv2", [128, 1], stack=p1)
            S.op("dve", lambda e: e.tensor_scalar_mul(inv2[:], inv[:], 1.0 / (2 * PI)), r=[Binv], w=[Binv2])
            blocks = [("own", i * 512, 512) for i in range(4)] + [("aux", i * 512, 512) for i in range(4)] + \
                     [("aux", 2048, 256)]
            SC = 2 * PI * (1.0 - 1e-7)

            def sin_of(out_ap, Bout, n, shift):
                S.op("dve", lambda e: e.tensor_scalar(a1[:, 0:n], ang[:, 0:n], inv2[:, 0:1], shift, ALU.mult, ALU.add),
                     r=[Bang, Binv2], w=[Ba1])
                S.op("dve", lambda e: e.tensor_copy(ai[:, 0:n], a1[:, 0:n]), r=[Ba1], w=[Bai])
                S.op("dve", lambda e: e.tensor_copy(a2[:, 0:n], ai[:, 0:n]), r=[Bai], w=[Ba2])
                S.op("dve", lambda e: e.tensor_tensor(a1[:, 0:n], a1[:, 0:n], a2[:, 0:n], ALU.subtract),
                     r=[Ba1, Ba2], w=[Ba1])
                S.op("dve", lambda e: e.tensor_single_scalar(a2[:, 0:n], a1[:, 0:n], 0.5, ALU.is_gt),
                     r=[Ba1], w=[Ba2])
                S.op("dve", lambda e: e.tensor_tensor(a1[:, 0:n], a1[:, 0:n], a2[:, 0:n], ALU.subtract),
                     r=[Ba1, Ba2], w=[Ba1])
                S.op("act", lambda e: e.activation(out_ap, a1[:, 0:n], AF.Sin, scale=SC), r=[Ba1], w=[Bout])

            def rope_block(kind, c0, n):
                src = ango_d if kind == "own" else anga_d
                S.dma("sp", ang[:, 0:n], src[:, c0:c0 + n], Bang, w=[Bang])
                s_out, Bs_out = (sino[:, c0:c0 + n], Bsino) if kind == "own" else (so[:, 0:n], Bso)
                c_out, Bc_out = (coso[:, c0:c0 + n], Bcoso) if kind == "own" else (co[:, 0:n], Bco)
                sin_of(s_out, Bs_out, n, 0.0)
                sin_of(c_out, Bc_out, n, 0.25)
                if kind == "aux":
                    S.dma("act", sina_d[:, c0:c0 + n], so[:, 0:n], Bso, r=[Bso], w=[daux[NTA + daux_n[0]]])
                    S.dma("act", cosa_d[:, c0:c0 + n], co[:, 0:n], Bco, r=[Bco], w=[daux[NTA + 10 + daux_n[0]]])
                    daux_n[0] += 1

            rope_todo = list(blocks)
            wad2, Bwad2 = sb("wad2", [128, 8, 1536], BF16, stack=p1)
            barow2, Bbarow2 = sb("barow2", [128, 2, D], stack=p1)
            S.dma("sp", barow2[:, 1, :], b_ada[:, 5 * D:6 * D].partition_broadcast(128), Bbarow2, w=[Bbarow2])

            def ada_late(piece):
                ada_piece(piece, wad2, Bwad2, PS[4][0], PS[4][1], 24, [PS[5], PS[5]], barow2, Bbarow2)
                if piece == 3:
                    S.op("dve", lambda e: e.tensor_tensor(
                        modB[:], PS[4][0][:, 0:48].rearrange("p (j c) -> p j c", c=2),
                        fm[:, R_BADA + 24:R_BADA + 48].unsqueeze(2).to_broadcast([128, 24, 2]), ALU.add),
                        r=[PS[4][1], Bfm], w=[BmodB])
                    S.op("dve", lambda e: e.tensor_scalar_add(sc1f[:], modB[:, 8:16, 0], 1.0), r=[BmodB], w=[Bsc1f])
            tiles = [("own", i) for i in range(NT)] + [("aux", i) for i in range(NTA)] + [("halo", 0)]
            for n, (kind, i) in enumerate(tiles):
                xtt, Bxt = xt[n % 4]
                rows = 32 if kind == "halo" else 128
                src = {"own": xo, "aux": xa, "halo": xh}[kind]
                r0 = 0 if kind == "halo" else i * 128
                S.dma("sp", xtt[0:rows, :], src[r0:r0 + rows, :], Bxt, w=[Bxt])
                which = 1 if (kind == "aux" and i >= 16) else 0
                pa, Bpa = PS[(n % 2) * 2]
                pb, Bpb = PS[(n % 2) * 2 + 1]
                for dt in range(8):
                    pp, Bpp = (pa, Bpa) if dt < 4 else (pb, Bpb)
                    S.op("pe", lambda e, dt=dt, pp=pp, rows=rows, xtt=xtt: e.transpose(
                        pp[:, (dt % 4) * 128:(dt % 4) * 128 + rows], xtt[0:rows, dt * 128:(dt + 1) * 128],
                        ident[0:rows, 0:rows]), r=[Bxt, Bcst], w=[Bpp])
                if kind == "aux":
                    dst, Bdst = ust[i % 2]
                if kind == "own":
                    o3, Bo = u_own[:, :, i * 128:(i + 1) * 128], Bu
                elif kind == "halo":
                    o3, Bo = u_halo[:, :, :], Buh
                else:
                    o3, Bo = dst[:, :, :], Bdst
                for dt in range(4):
                    S.op("act", lambda e, o3=o3, pa=pa, dt=dt, rows=rows, which=which: e.activation(
                        o3[:, dt, :], pa[:, dt * 128:dt * 128 + rows], AF.Identity,
                        bias=modfm[:, dt, which:which + 1], scale=sc1m[:, dt, which:which + 1]),
                        r=[Bpa, Bmod, Bsc1m], w=[Bo])
                tq, Btq = utmp[n % 2]
                pb3 = pb[:].rearrange("p (a b) -> p a b", a=4)[:, :, 0:rows]
                S.op("dve", lambda e, tq=tq, pb3=pb3, rows=rows, which=which: e.tensor_tensor(
                    tq[:, :, 0:rows], pb3, sc1m[:, 4:8, which:which + 1].to_broadcast([128, 4, rows]), ALU.mult),
                    r=[Bpb, Bsc1m], w=[Btq])
                S.op("dve", lambda e, tq=tq, o3=o3, rows=rows, which=which: e.tensor_tensor(
                    o3[:, 4:8, :], tq[:, :, 0:rows], modfm[:, 4:8, which:which + 1].to_broadcast([128, 4, rows]), ALU.add),
                    r=[Btq, Bmod], w=[Bo])
                if kind == "aux":
                    S.dma("act", uaux_d[:, :, i * 128:(i + 1) * 128], dst[:], Bdst, r=[Bdst], w=[daux[i]])
                if n % 4 == 1 and rope_todo:
                    rope_block(*rope_todo.pop(0))
                if n == 5:
                    ada_late(2)
                if n == 13:
                    ada_late(3)

            while rope_todo:
                rope_block(*rope_todo.pop(0))
        if stage == "p1":
            dbg_dump(sino[:], Bsino, 128, 2048, 0)
            dbg_dump(coso[:], Bcoso, 128, 2048, 2048)
            with scope() as pd:
                tmpf, Btmpf = sb("tmpf", [128, 4096], stack=pd)
                S.op("dve", lambda e: e.tensor_copy(tmpf[:, 0:2048], u_own[:, 0, :]), r=[Bu], w=[Btmpf])
                S.op("dve", lambda e: e.tensor_copy(tmpf[:, 2048:4096], u_own[:, 7, :]), r=[Bu], w=[Btmpf])
                dbg_dump(tmpf[:], Btmpf, 128, 4096, 4096)
                S.wait_all("sp", [Btmpf, Bsino, Bcoso])
                S.wait_all("sp", list(all_bufs))
            p12cm.__exit__(None, None, None)
            return nc

        with scope() as p2:
            def sb2(name, shape, dt=F32):
                return sb(name, shape, dt, stack=p2)
            qT, BqT = sb2("qT", [128, T], BF16)
            kT, BkT = sb2("kT", [128, T], BF16)
            ktf, Bktf = sb2("ktf", [128, NT, 128], BF16)
            ktb, Bktb = sb2("ktb", [128, NT, 128], BF16)
            vv, Bvv = sb2("vv", [128, NT, 256], BF16)
            sg, Bsg = sb2("sg", [128, NT, 256], BF16)
            Sf16, BSf16 = sb2("Sf16", [128, NT, 256], BF16)
            Sb16, BSb16 = sb2("Sb16", [128, NT, 256], BF16)
            retT, BretT = sb2("retT", [128, 2, T], BF16)
            maskT, BmaskT = sb2("maskT", [128, 128])
            qdf, Bqdf = sb2("qdf", [128, 128])
            qdb, Bqdb = sb2("qdb", [128, 128])
            m1, Bm1 = sb2("m1", [128, 128])
            m2, Bm2 = sb2("m2", [128, 128])
            UA = [sb2("ua%d" % i, [128, 8, 512], BF16) for i in range(2)]
            SA = [sb2("sa%d" % i, [128, 512]) for i in range(2)]
            CA = [sb2("ca%d" % i, [128, 512]) for i in range(2)]
            KTA = [sb2("kta%d" % i, [128, 4, 128], BF16) for i in range(2)]
            VA = [sb2("va%d" % i, [128, 2, 512], BF16) for i in range(2)]
            QB = [sb2("qb%d" % i, [128, 512], BF16) for i in range(2)]
            T1 = [sb2("t1_%d" % i, [128, 512]) for i in range(2)]
            T2 = [sb2("t2_%d" % i, [128, 512]) for i in range(2)]
            Sst = [sb2("Sst%d" % i, [128, 256]) for i in range(2)]
            OT = [sb2("ot%d" % i, [128, 256]) for i in range(2)]
            R16 = [sb2("r16_%d" % i, [128, 256], BF16) for i in range(2)]
            STT = [sb2("stt%d" % i, [128, 6]) for i in range(2)]
            MV = [sb2("mv%d" % i, [128, 2]) for i in range(2)]
            RS = [sb2("rs%d" % i, [128, 1]) for i in range(2)]
            SM = [sb2("sm%d" % i, [128, 128], BF16) for i in range(2)]
            QF = [sb2("qf%d" % i, [128, 128], BF16) for i in range(2)]
            QBk = [sb2("qbk%d" % i, [128, 128], BF16) for i in range(2)]

            ridx = [0]
            pend = []
            two_pr = [False]

            def rope_submit(wt, Bwt, rhs_fn, Brhs, n, bias_col, cos_ap, sin_ap, Btabs, out_ap, Bout):
                i = ridx[0] % 2
                ridx[0] += 1
                pq, Bpq = PS[i]
                pr, Bpr = PS[2 + i] if two_pr[0] else PS[2]
                qb, Bqb = QB[i]; t1, Bt1 = T1[i]; t2, Bt2 = T2[i]
                for k in range(8):
                    mm(pq[:, 0:n], wt[:, k, :], rhs_fn(k), k == 0, k == 7, [Bwt, Brhs], [Bpq])
                bias = fm[:, bias_col:bias_col + 1]
                S.op("act", lambda e: e.activation(qb[:, 0:n], pq[:, 0:n], AF.Identity, bias=bias),
                     r=[Bpq, Bfm], w=[Bqb])
                S.op("dve", lambda e: e.scalar_tensor_tensor(out=t1[:, 0:n], in0=pq[:, 0:n], scalar=bias, in1=cos_ap,
                                                             op0=ALU.add, op1=ALU.mult),
                     r=[Bpq, Bfm] + Btabs, w=[Bt1])

                def stage2():
                    mm(pr[:, 0:n], protb, qb[:, 0:n], True, True, [Bcstb, Bqb], [Bpr])
                    S.op("dve", lambda e: e.tensor_tensor(t2[:, 0:n], pr[:, 0:n], sin_ap, ALU.mult),
                         r=[Bpr] + Btabs, w=[Bt2])
                    S.op("pool", lambda e: e.tensor_tensor(out_ap, t1[:, 0:n], t2[:, 0:n], ALU.add),
                         r=[Bt1, Bt2], w=[Bout])
                pend.append(stage2)
                if len(pend) > 1:
                    pend.pop(0)()

            def rope_flush():
                while pend:
                    pend.pop(0)()

            KTAB = [sb2("kTa%d" % i, [128, 512], BF16) for i in range(2)]
            NH = int(os.environ.get('NHEADS', H))
            for h in range(NH):
                Wd = W[h % 2]
                wq, Bwq = Wd["wq"]; wk, Bwk = Wd["wk"]; wvg, Bwvg = Wd["wvg"]; bvg, Bbvg = Wd["bvg"]
                if h + 1 < NH:
                    load_head_w(h + 1)
                lgf = lg[:, h:h + 1]
                lgb = lg[:, 8 + h:9 + h]
                S.op("act", lambda e: e.activation(m1[:], cst[:, C_DF:C_DF + 128], AF.Exp, scale=lgf),
                     r=[Bcst, Blg], w=[Bm1])
                S.op("act", lambda e: e.activation(m2[:], cst[:, C_DB:C_DB + 128], AF.Exp, scale=lgb),
                     r=[Bcst, Blg], w=[Bm2])
                S.op("dve", lambda e: e.scalar_tensor_tensor(out=m1[:], in0=m1[:], scalar=KSCALE,
                                                             in1=cst[:, C_MF:C_MF + 128], op0=ALU.mult, op1=ALU.mult),
                     r=[Bm1, Bcst], w=[Bm1])
                S.op("dve", lambda e: e.scalar_tensor_tensor(out=m2[:], in0=m2[:], scalar=KSCALE,
                                                             in1=cst[:, C_MB:C_MB + 128], op0=ALU.mult, op1=ALU.mult),
                     r=[Bm2, Bcst], w=[Bm2])
                S.op("dve", lambda e: e.tensor_tensor(maskT[:], m1[:], m2[:], ALU.add), r=[Bm1, Bm2], w=[BmaskT])
                S.op("act", lambda e: e.activation(qdf[:], cst[:, C_I1:C_I1 + 128], AF.Exp, scale=lgf),
                     r=[Bcst, Blg], w=[Bqdf])
                S.op("act", lambda e: e.activation(qdb[:], cst[:, C_I2:C_I2 + 128], AF.Exp, scale=lgb),
                     r=[Bcst, Blg], w=[Bqdb])

                psfb, Bpsfb = PS[5]
                ablocks = [(i * 512, 512) for i in range(4)] + [(2048, 256)]

                def aux_load(bi):
                    c0, n = ablocks[bi]
                    ua, Bua = UA[bi % 2]; sa, Bsa = SA[bi % 2]; ca, Bca = CA[bi % 2]
                    S.dma("sp", ua[:, :, 0:n], uaux_d[:, :, c0:c0 + n], Bua, r=daux, w=[Bua])
                    S.dma("sp", sa[:, 0:n], sina_d[:, c0:c0 + n], Bsa, r=daux, w=[Bsa])
                    S.dma("sp", ca[:, 0:n], cosa_d[:, c0:c0 + n], Bca, r=daux, w=[Bca])

                def aux_rope(bi):
                    c0, n = ablocks[bi]
                    ua, Bua = UA[bi % 2]; sa, Bsa = SA[bi % 2]; ca, Bca = CA[bi % 2]
                    kTa, BkTa = KTAB[bi % 2]
                    rope_submit(wk, Bwk, lambda k, ua=ua, n=n: ua[:, k, 0:n], Bua, n, R_BIN + 8 + h,
                                ca[:, 0:n], sa[:, 0:n], [Bsa, Bca], kTa[:, 0:n], BkTa)

                two_pr[0] = False
                aux_load(0)
                aux_rope(0)
                stage2q = []
                ones1 = cstb[0:1, CB_ON:CB_ON + 128]
                pvi = [0]

                def aux_block(bi):
                    c0, n = ablocks[bi]
                    nt_ = n // 128
                    ua, Bua = UA[bi % 2]
                    kTa, BkTa = KTAB[bi % 2]
                    pb_, Bpb_ = PSBs[bi % 2]
                    kta, Bkta = KTA[bi % 2]
                    for pr2 in range(nt_ // 2):
                        pv, Bpv = PS[4] if pvi[0] % 2 == 0 else PS[3]
                        va, Bva = VA[pvi[0] % 2]
                        pvi[0] += 1
                        t0_ = c0 // 128 + pr2 * 2
                        for j in range(2):
                            tt = pr2 * 2 + j
                            for k in range(8):
                                mm(pv[:, j * 256:(j + 1) * 256], ua[:, k, tt * 128:(tt + 1) * 128], wvg[:, k, 0:256], k == 0, False,
                                   [Bua, Bwvg], [Bpv])
                            mm(pv[:, j * 256:(j + 1) * 256], ones1, bvg[0:1, 0:256], False, True, [Bcstb, Bbvg], [Bpv])
                        if pr2 == 0:
                            for tt in range(nt_):
                                S.op("pe", lambda e, tt=tt: e.transpose(pb_[:, tt * 128:(tt + 1) * 128], kTa[:, tt * 128:(tt + 1) * 128], identb),
                                     r=[BkTa, Bcstb], w=[Bpb_])
                            S.op("act", lambda e: e.activation(kta[:, 0:nt_, :], pb_[:, 0:n].rearrange("p (a b) -> p a b", b=128), AF.Copy),
                                 r=[Bpb_], w=[Bkta])
                        pv3 = pv[:].rearrange("p (a b) -> p a b", a=2)
                        S.op("dve", lambda e, va=va, pv3=pv3, t0_=t0_: e.tensor_tensor(
                            va[:, :, 0:256], pv3, waux[:, t0_:t0_ + 2, h:h + 1].to_broadcast([128, 2, 256]), ALU.mult),
                            r=[Bpv, Bwaux], w=[Bva])
                        S.op("dve", lambda e, va=va, pv3=pv3, t0_=t0_: e.tensor_tensor(
                            va[:, :, 256:512], pv3, waux[:, t0_:t0_ + 2, 8 + h:9 + h].to_broadcast([128, 2, 256]), ALU.mult),
                            r=[Bpv, Bwaux], w=[Bva])

                        def s2(va=va, Bva=Bva, t0_=t0_, pr2=pr2):
                            for j in range(2):
                                t = t0_ + j
                                mm(psfb[:], kta[:, pr2 * 2 + j, :], va[:, j, :], t == 0, t == NTA - 1, [Bkta, Bva], [Bpsfb])
                        stage2q.append(s2)
                        if len(stage2q) > 1:
                            stage2q.pop(0)()

                for bi, (c0, n) in enumerate(ablocks):
                    if bi + 1 < len(ablocks):
                        aux_load(bi + 1)
                    rope_flush()
                    aux_block(bi)
                    if bi + 1 < len(ablocks):
                        aux_rope(bi + 1)
                while stage2q:
                    stage2q.pop(0)()
                sf, Bsf = Sst[0]
                sbk, Bsbk = Sst[1]
                S.op("dve", lambda e: e.tensor_copy(sf[:], psfb[:, 0:256]), r=[Bpsfb], w=[Bsf])
                S.op("dve", lambda e: e.tensor_copy(sbk[:], psfb[:, 256:512]), r=[Bpsfb], w=[Bsbk])

                two_pr[0] = True
                for blk in range(4):
                    cs = slice(blk * 512, (blk + 1) * 512)
                    rope_submit(wk, Bwk, lambda k, cs=cs: u_own[:, k, cs], Bu, 512, R_BIN + 8 + h,
                                coso[:, cs], sino[:, cs], [Bsino, Bcoso], kT[:, cs], BkT)
                    rope_submit(wq, Bwq, lambda k, cs=cs: u_own[:, k, cs], Bu, 512, R_BIN + h,
                                coso[:, cs], sino[:, cs], [Bsino, Bcoso], qT[:, cs], BqT)
                rope_flush()
                for c in range(NT):
                    pv, Bpv = PS[c % 2 + 4]
                    for k in range(8):
                        mm(pv[:], u_own[:, k, c * 128:(c + 1) * 128], wvg[:, k, :], k == 0, False, [Bu, Bwvg], [Bpv])
                    mm(pv[:], ones1, bvg[0:1, :], False, True, [Bcstb, Bbvg], [Bpv])
                    pb_, Bpb_ = PSBs[c % 2]
                    S.op("pe", lambda e, c=c, pb_=pb_: e.transpose(
                        pb_[:, 0:128], kT[:, c * 128:(c + 1) * 128], identb), r=[BkT, Bcstb], w=[Bpb_])
                    S.op("act", lambda e, c=c, pb_=pb_: e.activation(
                        ktf[:, c, :], pb_[:, 0:128], AF.Copy, scale=kdec[:, h:h + 1]), r=[Bpb_, Bkdec], w=[Bktf])
                    S.op("act", lambda e, c=c, pb_=pb_: e.activation(
                        ktb[:, c, :], pb_[:, 0:128], AF.Copy, scale=kdec[:, 8 + h:9 + h]), r=[Bpb_, Bkdec], w=[Bktb])
                    S.op("dve", lambda e, c=c, pv=pv: e.tensor_copy(vv[:, c, :], pv[:, 0:256]), r=[Bpv], w=[Bvv])
                    S.op("act", lambda e, c=c, pv=pv: e.activation(sg[:, c, :], pv[:, 256:512], AF.Silu), r=[Bpv], w=[Bsg])
                S.op("act", lambda e: e.activation(Sf16[:, 0, :], sf[:], AF.Copy), r=[Bsf], w=[BSf16])
                S.op("act", lambda e: e.activation(Sb16[:, NT - 1, :], sbk[:], AF.Copy), r=[Bsbk], w=[BSb16])
                for c in range(NT - 1):
                    pd_, Bpd = PS[4 + c % 2]
                    mm(pd_[:, 0:256], ktf[:, c, :], vv[:, c, :], True, True, [Bktf, Bvv], [Bpd])
                    cb_ = NT - 1 - c
                    mm(pd_[:, 256:512], ktb[:, cb_, :], vv[:, cb_, :], True, True, [Bktb, Bvv], [Bpd])
                    S.op("dve", lambda e, pd_=pd_: e.scalar_tensor_tensor(
                        out=sf[:], in0=sf[:], scalar=cdec[:, h:h + 1], in1=pd_[:, 0:256], op0=ALU.mult, op1=ALU.add),
                        r=[Bsf, Bcdec, Bpd], w=[Bsf])
                    S.op("dve", lambda e, pd_=pd_: e.scalar_tensor_tensor(
                        out=sbk[:], in0=sbk[:], scalar=cdec[:, 8 + h:9 + h], in1=pd_[:, 256:512], op0=ALU.mult,
                        op1=ALU.add), r=[Bsbk, Bcdec, Bpd], w=[Bsbk])
                    S.op("act", lambda e, c=c: e.activation(Sf16[:, c + 1, :], sf[:], AF.Copy), r=[Bsf], w=[BSf16])
                    S.op("act", lambda e, cb_=cb_: e.activation(Sb16[:, cb_ - 1, :], sbk[:], AF.Copy),
                         r=[Bsbk], w=[BSb16])

                def sw_a(c):
                    cs = slice(c * 128, (c + 1) * 128)
                    i2 = c % 2
                    psc, Bpsc = PS[i2]
                    sm, Bsm = SM[i2]; qf, Bqf = QF[i2]; qbk, Bqbk = QBk[i2]
                    mm(psc[:, 0:128], kT[:, cs], qT[:, cs], True, True, [BkT, BqT], [Bpsc])
                    S.op("dve", lambda e: e.tensor_tensor(sm[:], psc[:, 0:128], maskT[:], ALU.mult),
                         r=[Bpsc, BmaskT], w=[Bsm])
                    S.op("pool", lambda e: e.tensor_tensor(qf[:], qT[:, cs], qdf[:], ALU.mult), r=[BqT, Bqdf], w=[Bqf])
                    S.op("pool", lambda e: e.tensor_tensor(qbk[:], qT[:, cs], qdb[:], ALU.mult), r=[BqT, Bqdb], w=[Bqbk])

                def sw_b(c):
                    i2 = c % 2
                    po, Bpo = PS[2 + i2]
                    sm, Bsm = SM[i2]; qf, Bqf = QF[i2]; qbk, Bqbk = QBk[i2]
                    mm(po[:, 0:256], sm[:], vv[:, c, :], True, False, [Bsm, Bvv], [Bpo])
                    mm(po[:, 0:256], qf[:], Sf16[:, c, :], False, False, [Bqf, BSf16], [Bpo])
                    mm(po[:, 0:256], qbk[:], Sb16[:, c, :], False, True, [Bqbk, BSb16], [Bpo])
                    st_, Bst = STT[i2]; mv, Bmv = MV[i2]; rs, Brs = RS[i2]; ot, Bot = OT[i2]; r16, Br16 = R16[i2]
                    S.op("dve", lambda e: e.bn_stats(st_[:], po[:, 0:256]), r=[Bpo], w=[Bst])
                    S.op("dve", lambda e: e.bn_aggr(mv[:], st_[:]), r=[Bst], w=[Bmv])
                    S.op("act", lambda e: e.activation(rs[:], mv[:, 1:2], AF.Sqrt, bias=epsb[:, 0:1]),
                         r=[Bmv, Bepsb], w=[Brs])
                    S.op("dve", lambda e: e.scalar_tensor_tensor(
                        out=ot[:], in0=po[:, 0:256], scalar=mv[:, 0:1], in1=sg[:, c, :], op0=ALU.subtract, op1=ALU.mult),
                        r=[Bpo, Bmv, Bsg], w=[Bot])
                    S.op("dve", lambda e: e.reciprocal(rs[:], rs[:]), r=[Brs], w=[Brs])
                    S.op("act", lambda e: e.activation(r16[:], ot[:], AF.Copy, scale=rs[:, 0:1]),
                         r=[Bot, Brs], w=[Br16])

                def sw_c(c):
                    cs = slice(c * 128, (c + 1) * 128)
                    i2 = c % 2
                    r16, Br16 = R16[i2]
                    pb_, Bpb_ = PSBs[i2]
                    for vt in range(2):
                        S.op("pe", lambda e, vt=vt: e.transpose(
                            pb_[:, vt * 128:(vt + 1) * 128], r16[:, vt * 128:(vt + 1) * 128], identb),
                            r=[Br16, Bcstb], w=[Bpb_])
                    S.op("act", lambda e: e.activation(
                        retT[:, :, cs], pb_[:, 0:256].rearrange("p (a b) -> p a b", a=2), AF.Copy),
                        r=[Bpb_], w=[BretT])

                for s_ in range(NT + 2):
                    if s_ < NT:
                        sw_a(s_)
                    if 1 <= s_ <= NT:
                        sw_b(s_ - 1)
                    if s_ >= 2:
                        sw_c(s_ - 2)
                S.dma("act", retT_d[:, 2 * h:2 * h + 2, :], retT[:], BretT, r=[BretT], w=[dbuf_ret])
                if stage == "p2" and h in (0, 7):
                    with scope() as pd:
                        tmpf, Btmpf = sb("tmpf%d" % h, [128, 1024], stack=pd)
                        for a in range(2):
                            for q4 in range(2):
                                S.op("dve", lambda e, a=a, q4=q4: e.tensor_copy(tmpf[:], retT[:, a, q4 * 1024:(q4 + 1) * 1024]),
                                     r=[BretT], w=[Btmpf])
                                dbg_dump(tmpf[:], Btmpf, 128, 1024, (0 if h == 0 else 4096) + a * 2048 + q4 * 1024)
                        S.wait_all("sp", [Btmpf])
            if stage == "p2":
                print("counts", S.cnt, S.nd)
                S.wait_all("sp", [dbuf, BretT])
                return nc

        p12cm.__exit__(None, None, None)

        def stopat(tag):
            if os.environ.get("STOPAT") == tag:
                S.wait_all("sp", list(all_bufs))
                return True
            return False

        Ycm = scope(); Yst = Ycm.__enter__()
        comb, Bcomb = sb("comb", [128, 8, T], BF16, stack=Yst)
        Zcm = scope(); Zst = Zcm.__enter__()
        z2, Bz2 = sb("z2", [128, 8, T], BF16, stack=Zst)
        ZW = T + 32
        with scope() as p3a:
            zext, Bzext = sb("zext", [128, 8, ZW], BF16, stack=p3a)
            WZ = [sb("wz%d" % i, [128, 8, 256], BF16, stack=p3a) for i in range(2)]
            SIG = [sb("sig%d" % i, [128, 512], stack=p3a) for i in range(2)]
            zh, Bzh = sb("zh", [128, 32], stack=p3a)
            DG = [sb("dg%d" % i, [128, 31, 128], BF16, stack=p3a) for i in range(2)]
            it = 0
            for ct in range(8):
                wz, Bwz = WZ[ct % 2]
                c_a = GLU_OFF + ct * 128
                c_b = GLU_OFF + 1024 + ct * 128
                S.dma("pool", wz[:, :, 0:128], w_in[:, c_a:c_a + 128].rearrange("(k p) c -> p k c", p=128), Bwz, w=[Bwz])
                S.dma("pool", wz[:, :, 128:256], w_in[:, c_b:c_b + 128].rearrange("(k p) c -> p k c", p=128), Bwz,
                      w=[Bwz], join=True)
                bza = fm[:, R_BIN + 48 + ct:R_BIN + 49 + ct]
                bzb = fm[:, R_BIN + 56 + ct:R_BIN + 57 + ct]
                segs = [("own", blk) for blk in range(4)] + [("halo", 0)]
                for kind, blk in segs:
                    n = 512 if kind == "own" else 32
                    pa, Bpa = PS[(it % 3) * 2]
                    pb, Bpb = PS[(it % 3) * 2 + 1]
                    sig, Bsig = SIG[it % 2]
                    it += 1
                    for k in range(8):
                        rhs = u_own[:, k, blk * 512:(blk + 1) * 512] if kind == "own" else u_halo[:, k, :]
                        mm(pa[:, 0:n], wz[:, k, 0:128], rhs, k == 0, k == 7, [Bwz, Bu, Buh], [Bpa])
                    for k in range(8):
                        rhs = u_own[:, k, blk * 512:(blk + 1) * 512] if kind == "own" else u_halo[:, k, :]
                        mm(pb[:, 0:n], wz[:, k, 128:256], rhs, k == 0, k == 7, [Bwz, Bu, Buh], [Bpb])
                    S.op("act", lambda e, sig=sig, pb=pb, n=n, bzb=bzb: e.activation(sig[:, 0:n], pb[:, 0:n], AF.Sigmoid, bias=bzb),
                         r=[Bpb, Bfm], w=[Bsig])
                    if kind == "own":
                        S.op("dve", lambda e, pa=pa, sig=sig, bza=bza, ct=ct, blk=blk: e.scalar_tensor_tensor(
                            out=zext[:, ct, 16 + blk * 512:16 + (blk + 1) * 512], in0=pa[:, 0:512], scalar=bza,
                            in1=sig[:, 0:512], op0=ALU.add, op1=ALU.mult), r=[Bpa, Bsig, Bfm], w=[Bzext])
                    else:
                        S.op("dve", lambda e, pa=pa, sig=sig, bza=bza: e.scalar_tensor_tensor(
                            out=zh[:], in0=pa[:, 0:32], scalar=bza, in1=sig[:, 0:32], op0=ALU.add, op1=ALU.mult),
                            r=[Bpa, Bsig, Bfm], w=[Bzh])
                        S.op("dve", lambda e, ct=ct: e.tensor_tensor(zext[:, ct, 0:16], zh[:, 0:16], cst[:, C_HM:C_HM + 16], ALU.mult),
                             r=[Bzh, Bcst], w=[Bzext])
                        S.op("dve", lambda e, ct=ct: e.tensor_tensor(zext[:, ct, ZW - 16:ZW], zh[:, 16:32],
                                                                  cst[:, C_HM + 16:C_HM + 32], ALU.mult),
                             r=[Bzh, Bcst], w=[Bzext])
            def build_diag(ct):
                dg, Bdg = DG[ct % 2]
                for t in range(31):
                    col = R_CW + t * 8 + ct
                    if t % 2 == 0:
                        S.op("dve", lambda e, dg=dg, t=t, col=col: e.tensor_scalar_mul(dg[:, t, :], identb, fm[:, col:col + 1]),
                             r=[Bcstb, Bfm], w=[Bdg])
                    else:
                        S.op("act", lambda e, dg=dg, t=t, col=col: e.activation(dg[:, t, :], identb, AF.Copy, scale=fm[:, col:col + 1]),
                             r=[Bcstb, Bfm], w=[Bdg])

            it = 0
            build_diag(0)
            for ct in range(8):
                dg, Bdg = DG[ct % 2]
                if ct + 1 < 8:
                    build_diag(ct + 1)
                for blk in range(4):
                    pc, Bpc = PS[it % 6]
                    it += 1
                    for t in range(31):
                        o0 = blk * 512 + t + 1
                        mm(pc[:], dg[:, t, :], zext[:, ct, o0:o0 + 512], t == 0, t == 30, [Bdg, Bzext], [Bpc])
                    S.op("act", lambda e, pc=pc, ct=ct, blk=blk: e.activation(
                        z2[:, ct, blk * 512:(blk + 1) * 512], pc[:], AF.Identity, bias=fm[:, R_CB + ct:R_CB + ct + 1]),
                        r=[Bpc, Bfm], w=[Bz2])
        if stage == "p3a":
            with scope() as pd:
                tmpf, Btmpf = sb("tmpfz", [128, 2048], stack=pd)
                for ct in (0, 7):
                    S.op("dve", lambda e, ct=ct: e.tensor_copy(tmpf[:], z2[:, ct, :]), r=[Bz2], w=[Btmpf])
                    dbg_dump(tmpf[:], Btmpf, 128, 2048, 0 if ct == 0 else 2048)
                S.wait_all("sp", [Btmpf])
            Zcm.__exit__(None, None, None); Ycm.__exit__(None, None, None)
            return nc
        with scope() as p3b:
            sq, Bsq = sb("sq", [128, 8, 512], BF16, stack=p3b)
            mean, Bmean = sb("mean", [128, 512], stack=p3b)
            var, Bvar = sb("var", [128, 512], stack=p3b)
            TMP = [sb("lnt%d" % i, [128, 512], stack=p3b) for i in range(2)]
            for blk in range(4):
                bs = slice(blk * 512, (blk + 1) * 512)
                S.op("act", lambda e, bs=bs: e.activation(sq[:], z2[:, :, bs], AF.Square), r=[Bz2], w=[Bsq])
                pS, BpS = PS[0]
                pQ, BpQ = PS[1]
                for ct in range(8):
                    mm(pS[:], onesb, z2[:, ct, bs], ct == 0, ct == 7, [Bcstb, Bz2], [BpS])
                for ct in range(8):
                    mm(pQ[:], onesb, sq[:, ct, :], ct == 0, ct == 7, [Bcstb, Bsq], [BpQ])
                S.op("act", lambda e: e.activation(mean[:], pS[:], AF.Copy, scale=1.0 / D), r=[BpS], w=[Bmean])
                S.op("dve", lambda e: e.tensor_tensor(var[:], mean[:], mean[:], ALU.mult), r=[Bmean], w=[Bvar])
                S.op("dve", lambda e: e.scalar_tensor_tensor(out=var[:], in0=pQ[:], scalar=1.0 / D, in1=var[:],
                                                             op0=ALU.mult, op1=ALU.subtract), r=[BpQ, Bvar], w=[Bvar])
                S.op("act", lambda e: e.activation(var[:], var[:], AF.Sqrt, bias=epsb[:, 0:1]), r=[Bvar, Bepsb], w=[Bvar])
                S.op("dve", lambda e: e.reciprocal(var[:], var[:]), r=[Bvar], w=[Bvar])
                for ct in range(8):
                    tm, Btm = TMP[ct % 2]
                    S.op("dve", lambda e, tm=tm, ct=ct, bs=bs: e.tensor_tensor(tm[:], z2[:, ct, bs], mean[:], ALU.subtract),
                         r=[Bz2, Bmean], w=[Btm])
                    S.op("dve", lambda e, tm=tm: e.tensor_tensor(tm[:], tm[:], var[:], ALU.mult), r=[Btm, Bvar], w=[Btm])
                    S.op("act", lambda e, tm=tm, ct=ct, bs=bs: e.activation(
                        z2[:, ct, bs], tm[:], AF.Silu, bias=fm[:, R_LB + ct:R_LB + ct + 1], scale=fm[:, R_LG + ct:R_LG + ct + 1]),
                        r=[Btm, Bfm], w=[Bz2])
        if stage == "p3b":
            with scope() as pd:
                tmpf, Btmpf = sb("tmpfz", [128, 2048], stack=pd)
                for ct in (0, 7):
                    S.op("dve", lambda e, ct=ct: e.tensor_copy(tmpf[:], z2[:, ct, :]), r=[Bz2], w=[Btmpf])
                    dbg_dump(tmpf[:], Btmpf, 128, 2048, 0 if ct == 0 else 2048)
                S.wait_all("sp", [Btmpf])
            Zcm.__exit__(None, None, None); Ycm.__exit__(None, None, None)
            return nc

        with scope() as p3c:
            RB = [sb("rb%d" % i, [128, 16, 512], BF16, stack=p3c) for i in range(2)]
            WJ = [dict(wro=sb("wro%d" % i, [128, 2, 16, 128], BF16, stack=p3c), wco=sb("wco%d" % i, [128, 2, 8, 128], BF16, stack=p3c),
                       wg=sb("wgj%d" % i, [128, 2, 8, 256], BF16, stack=p3c)) for i in range(2)]
            GA = [sb("ga%d" % i, [128, 512], stack=p3c) for i in range(2)]
            GB = [sb("gb%d" % i, [128, 512], stack=p3c) for i in range(2)]
            TA = [sb("ta%d" % i, [128, 512], stack=p3c) for i in range(2)]
            TB = [sb("tb%d" % i, [128, 512], stack=p3c) for i in range(2)]
            it = 0
            ib = 0
            for jp in range(4):
                wro, Bwro = WJ[jp % 2]["wro"]; wco, Bwco = WJ[jp % 2]["wco"]; wgj, Bwgj = WJ[jp % 2]["wg"]
                for jj in range(2):
                    j = jp * 2 + jj
                    js = slice(j * 128, (j + 1) * 128)
                    S.dma("pool", wro[:, jj, :, :], w_ro[:, js].rearrange("(k p) c -> p k c", p=128), Bwro, w=[Bwro], join=(jj > 0))
                    S.dma("pool", wco[:, jj, :, :], w_co[:, js].rearrange("(k p) c -> p k c", p=128), Bwco, w=[Bwco], join=(jj > 0))
                    S.dma("pool", wgj[:, jj, :, 0:128], w_in[:, GATE_OFF + j * 128:GATE_OFF + (j + 1) * 128].rearrange(
                        "(k p) c -> p k c", p=128), Bwgj, w=[Bwgj], join=(jj > 0))
                    S.dma("pool", wgj[:, jj, :, 128:256], w_in[:, GATE_OFF + 1024 + j * 128:GATE_OFF + 1024 + (j + 1) * 128].rearrange(
                        "(k p) c -> p k c", p=128), Bwgj, w=[Bwgj], join=True)
                for blk in range(4):
                    bs = slice(blk * 512, (blk + 1) * 512)
                    rb, Brb = RB[ib % 2]
                    ib += 1
                    S.dma("sp", rb[:], retT_d[:, :, bs], Brb, r=[dbuf_ret], w=[Brb])
                    for jj in range(2):
                        j = jp * 2 + jj
                        i2 = it % 2
                        it += 1
                        pga, Bpga = PS[4]
                        pgb, Bpgb = PS[5]
                        pya, Bpya = PS[i2 * 2]
                        pyb, Bpyb = PS[i2 * 2 + 1]
                        ga, Bga = GA[i2]; gb, Bgb = GB[i2]; ta, Bta = TA[i2]; tb, Btb = TB[i2]
                        for k in range(8):
                            mm(pga[:], wgj[:, jj, k, 0:128], u_own[:, k, bs], k == 0, k == 7, [Bwgj, Bu], [Bpga])
                        for k in range(8):
                            mm(pgb[:], wgj[:, jj, k, 128:256], u_own[:, k, bs], k == 0, k == 7, [Bwgj, Bu], [Bpgb])
                        S.op("act", lambda e, ga=ga, pga=pga, j=j: e.activation(
                            ga[:], pga[:], AF.Sigmoid, bias=fm[:, R_BIN + 64 + j:R_BIN + 65 + j]), r=[Bpga, Bfm], w=[Bga])
                        S.op("act", lambda e, gb=gb, pgb=pgb, j=j: e.activation(
                            gb[:], pgb[:], AF.Sigmoid, bias=fm[:, R_BIN + 72 + j:R_BIN + 73 + j]), r=[Bpgb, Bfm], w=[Bgb])
                        for k in range(16):
                            mm(pya[:], wro[:, jj, k, :], rb[:, k, :], k == 0, k == 15, [Bwro, Brb], [Bpya])
                        for k in range(8):
                            mm(pyb[:], wco[:, jj, k, :], z2[:, k, bs], k == 0, k == 7, [Bwco, Bz2], [Bpyb])
                        S.op("dve", lambda e, ta=ta, pya=pya, ga=ga: e.tensor_tensor(ta[:], pya[:], ga[:], ALU.mult),
                             r=[Bpya, Bga], w=[Bta])
                        S.op("dve", lambda e, tb=tb, pyb=pyb, gb=gb, j=j: e.scalar_tensor_tensor(
                            out=tb[:], in0=pyb[:], scalar=fm[:, R_BCO + j:R_BCO + j + 1], in1=gb[:], op0=ALU.add, op1=ALU.mult),
                            r=[Bpyb, Bgb, Bfm], w=[Btb])
                        S.op("dve", lambda e, ta=ta, tb=tb, j=j, bs=bs: e.tensor_tensor(comb[:, j, bs], ta[:], tb[:], ALU.add),
                             r=[Bta, Btb], w=[Bcomb])
        Zcm.__exit__(None, None, None)
        if stage == "p3c":
            with scope() as pd:
                tmpf, Btmpf = sb("tmpfz", [128, 2048], stack=pd)
                for ct in (0, 7):
                    S.op("dve", lambda e, ct=ct: e.tensor_copy(tmpf[:], comb[:, ct, :]), r=[Bcomb], w=[Btmpf])
                    dbg_dump(tmpf[:], Btmpf, 128, 2048, 0 if ct == 0 else 2048)
                S.wait_all("sp", [Btmpf])
            Ycm.__exit__(None, None, None)
            return nc

        tTb, BtTb = u_own, Bu
        BIG = 1.0e4
        with scope() as p3d:
            wmo, Bwmo = sb("wmo", [128, 8, D], BF16, stack=p3d)
            S.dma("pool", wmo[:], w_mo.rearrange("(k p) c -> p k c", p=128), Bwmo, w=[Bwmo])
            rows = {}
            for nm, src in (("bmix", b_mo), ("l1g", ln1g), ("l1b", ln1b)):
                rows[nm] = sb(nm, [128, D], stack=p3d)
                S.dma("sp", rows[nm][0][:], src.partition_broadcast(128), rows[nm][1], w=[rows[nm][1]])
            wr32, Bwr32 = sb("wr32", [128, 8, 36], stack=p3d)
            S.dma("sp", wr32[:], w_rt.rearrange("(k p) c -> p k c", p=128), Bwr32, w=[Bwr32])
            brt, Bbrt = sb("brt", [128, 36], stack=p3d)
            S.dma("sp", brt[:], b_rt.partition_broadcast(128), Bbrt, w=[Bbrt])
            XT = [sb("x3_%d" % i, [128, D], stack=p3d) for i in range(2)]
            HP = [sb("hp%d" % i, [128, D], stack=p3d) for i in range(2)]
            TT32 = [sb("tt32_%d" % i, [128, 8, 128], stack=p3d) for i in range(2)]
            ST = [sb("st3_%d" % i, [128, 12], stack=p3d) for i in range(2)]
            MV3 = [sb("mv3_%d" % i, [128, 2], stack=p3d) for i in range(2)]
            RS3 = [sb("rs3_%d" % i, [128, 1], stack=p3d) for i in range(2)]
            Lall, BLall = sb("Lall", [128, NT, 36], stack=p3d)
            bmix, Bbmix = rows["bmix"]; l1g, Bl1g = rows["l1g"]; l1b, Bl1b = rows["l1b"]
            bmb, Bbmb = sb("bmb", [1, D], BF16, stack=p3d)
            S.dma("pool", bmb[:], b_mo, Bbmb, w=[Bbmb])
            gs8, Bgs8 = sb("gs8", [128, 8], stack=p3d)
            bs8, Bbs8 = sb("bs8", [128, 8], stack=p3d)
            S.op("dve", lambda e: e.tensor_tensor(gs8[:], fm[:, R_L1G:R_L1G + 8], sc1f[:], ALU.mult), r=[Bfm, Bsc1f], w=[Bgs8])
            S.op("dve", lambda e: e.tensor_tensor(bs8[:], fm[:, R_L1B:R_L1B + 8], sc1f[:], ALU.mult), r=[Bfm, Bsc1f], w=[Bbs8])
            S.op("dve", lambda e: e.tensor_tensor(bs8[:], bs8[:], modB[:, 0:8, 0], ALU.add), r=[Bbs8, BmodB], w=[Bbs8])
            NM = [sb("nmr%d" % i, [128, 1], stack=p3d) for i in range(2)]
            HN = [sb("hn%d" % i, [128, D], stack=p3d) for i in range(2)]

            def ln_norm(hp, Bhp, hn, Bhn, st_, Bst, mv, Bmv, rs, Brs, nm, Bnm):
                S.op("dve", lambda e: e.bn_stats(st_[:, 0:6], hp[:, 0:512]), r=[Bhp], w=[Bst])
                S.op("dve", lambda e: e.bn_stats(st_[:, 6:12], hp[:, 512:1024]), r=[Bhp], w=[Bst])
                S.op("dve", lambda e: e.bn_aggr(mv[:], st_[:]), r=[Bst], w=[Bmv])
                S.op("act", lambda e: e.activation(rs[:], mv[:, 1:2], AF.Ln, bias=epsb[:, 0:1]), r=[Bmv, Bepsb], w=[Brs])
                S.op("act", lambda e: e.activation(rs[:], rs[:], AF.Exp, scale=-0.5), r=[Brs], w=[Brs])
                S.op("dve", lambda e: e.scalar_tensor_tensor(out=nm[:], in0=mv[:, 0:1], scalar=-1.0, in1=rs[:],
                                                             op0=ALU.mult, op1=ALU.mult), r=[Bmv, Brs], w=[Bnm])
                S.op("act", lambda e: e.activation(hn[:], hp[:], AF.Identity, bias=nm[:, 0:1], scale=rs[:, 0:1]),
                     r=[Bhp, Bnm, Brs], w=[Bhn])

            def d_s1(tile):
                i2 = tile % 2
                ts_ = slice(tile * 128, (tile + 1) * 128)
                xt3, Bxt3 = XT[i2]; hp, Bhp = HP[i2]; hn, Bhn = HN[i2]
                S.dma("sp", xt3[:], xo[ts_, :], Bxt3, w=[Bxt3])
                for half in range(2):
                    pm, Bpm = PS[half]
                    hs = slice(half * 512, (half + 1) * 512)
                    for k in range(8):
                        mm(pm[:], comb[:, k, ts_], wmo[:, k, hs], k == 0, False, [Bcomb, Bwmo], [Bpm])
                    mm(pm[:], cstb[0:1, CB_ON:CB_ON + 128], bmb[0:1, hs], False, True, [Bcstb, Bbmb], [Bpm])
                    S.op("dve", lambda e, pm=pm, hs=hs: e.tensor_tensor(hp[:, hs], pm[:], gtrow[:, 0, hs], ALU.mult),
                         r=[Bpm, Bgtrow], w=[Bhp])
                S.op("dve", lambda e: e.scalar_tensor_tensor(out=hp[:], in0=xt3[:], scalar=ALPHA, in1=hp[:],
                                                             op0=ALU.mult, op1=ALU.add), r=[Bhp, Bxt3], w=[Bhp])

            Bst1 = [Buf("h1st0"), Buf("h1st1")]

            def d_s1b(tile):
                i2 = tile % 2
                ts_ = slice(tile * 128, (tile + 1) * 128)
                hp, Bhp = HP[i2]; hn, Bhn = HN[i2]
                st_, Bst = ST[i2]; mv, Bmv = MV3[i2]; rs, Brs = RS3[i2]; nm, Bnm = NM[i2]
                ln_norm(hp, Bhp, hn, Bhn, st_, Bst, mv, Bmv, rs, Brs, nm, Bnm)
                S.op("dve", lambda e: e.tensor_tensor(hp[:], hn[:], l1g[:], ALU.mult), r=[Bhn, Bl1g], w=[Bhp])
                S.op("dve", lambda e: e.tensor_tensor(hp[:], hp[:], l1b[:], ALU.add), r=[Bhp, Bl1b], w=[Bhp])
                S.dma("pool", h1_d[ts_, :], hp[:], Bst1[i2], r=[Bhp], w=[dbuf_h1])

            def d_s2(tile):
                i2 = tile % 2
                ts_ = slice(tile * 128, (tile + 1) * 128)
                hn, Bhn = HN[i2]; t32, Bt32 = TT32[i2]; hp, Bhp = HP[i2]
                pa, Bpa = PS[2]
                pb, Bpb = PS[3]
                pr_, Bpr_ = PS[4 + i2]
                for dt in range(8):
                    pp, Bpp = (pa, Bpa) if dt < 4 else (pb, Bpb)
                    S.op("pe", lambda e, dt=dt, pp=pp: e.transpose(
                        pp[:, (dt % 4) * 128:(dt % 4 + 1) * 128], hn[:, dt * 128:(dt + 1) * 128], ident), r=[Bhn, Bcst], w=[Bpp])
                for dt in range(8):
                    pp, Bpp = (pa, Bpa) if dt < 4 else (pb, Bpb)
                    S.op("act", lambda e, dt=dt, pp=pp: e.activation(
                        t32[:, dt, :], pp[:, (dt % 4) * 128:(dt % 4 + 1) * 128], AF.Identity,
                        bias=bs8[:, dt:dt + 1], scale=gs8[:, dt:dt + 1]), r=[Bpp, Bbs8, Bgs8], w=[Bt32])
                S.op("dve", lambda e: e.tensor_copy(tTb[:, :, ts_], t32[:]), r=[Bt32], w=[BtTb])
                for dt in range(8):
                    mm(pr_[:, 0:36], t32[:, dt, :], wr32[:, dt, :], dt == 0, dt == 7, [Bt32, Bwr32], [Bpr_])
                S.op("dve", lambda e: e.tensor_tensor(Lall[:, tile, :], pr_[:, 0:36], brt[:], ALU.add), r=[Bpr_, Bbrt], w=[BLall])
                if stage == "p3d" and tile in (0, 15):
                    dbg_dump(hp[:], Bhp, 128, 1024, 0 if tile == 0 else 1024)

            d_s1(0)
            d_s1b(0)
            for tile in range(NT):
                if tile + 1 < NT:
                    d_s1(tile + 1)
                d_s2(tile)
                if tile + 1 < NT:
                    d_s1b(tile + 1)

            X_ = mybir.AxisListType.X
            rt = {}
            for nm, shp in (("gmax", [128, NT]), ("eg", [128, NT, 4]), ("gw", [128, NT]), ("goh", [128, NT, 4]),
                            ("Lm", [128, NT, 32]), ("m1", [128, NT]), ("oh1", [128, NT, 32]), ("Lm2", [128, NT, 32]),
                            ("m2", [128, NT]), ("oh2", [128, NT, 32]), ("dd", [128, NT]), ("w1", [128, NT]), ("w2", [128, NT])):
                rt[nm] = sb("rt_" + nm, shp, stack=p3d)
            g = lambda nm: rt[nm][0]
            B_ = lambda nm: rt[nm][1]
            LG = Lall[:, :, 0:4]
            LE = Lall[:, :, 4:36]
            bc = lambda ap2, n: ap2.unsqueeze(2).to_broadcast([128, NT, n])
            S.op("dve", lambda e: e.tensor_reduce(g("gmax")[:], LG, X_, ALU.max), r=[BLall], w=[B_("gmax")])
            S.op("dve", lambda e: e.tensor_tensor(g("eg")[:], LG, bc(g("gmax")[:], 4), ALU.subtract), r=[BLall, B_("gmax")], w=[B_("eg")])
            S.op("act", lambda e: e.activation(g("eg")[:], g("eg")[:], AF.Exp), r=[B_("eg")], w=[B_("eg")])
            S.op("dve", lambda e: e.tensor_reduce(g("gw")[:], g("eg")[:], X_, ALU.add), r=[B_("eg")], w=[B_("gw")])
            S.op("dve", lambda e: e.reciprocal(g("gw")[:], g("gw")[:]), r=[B_("gw")], w=[B_("gw")])
            S.op("dve", lambda e: e.tensor_tensor(g("goh")[:], LG, bc(g("gmax")[:], 4), ALU.is_ge), r=[BLall, B_("gmax")], w=[B_("goh")])
            S.op("dve", lambda e: e.tensor_scalar(g("goh")[:], g("goh")[:], 1.0, BIG, ALU.subtract, ALU.mult), r=[B_("goh")], w=[B_("goh")])
            S.op("dve", lambda e: e.tensor_tensor(
                g("Lm")[:].rearrange("p t (a b) -> p t a b", a=4), LE.rearrange("p t (a b) -> p t a b", a=4),
                g("goh")[:].unsqueeze(3).to_broadcast([128, NT, 4, 8]), ALU.add), r=[BLall, B_("goh")], w=[B_("Lm")])
            S.op("dve", lambda e: e.tensor_reduce(g("m1")[:], g("Lm")[:], X_, ALU.max), r=[B_("Lm")], w=[B_("m1")])
            S.op("dve", lambda e: e.tensor_tensor(g("oh1")[:], g("Lm")[:], bc(g("m1")[:], 32), ALU.is_ge), r=[B_("Lm"), B_("m1")], w=[B_("oh1")])
            S.op("dve", lambda e: e.scalar_tensor_tensor(out=g("Lm2")[:], in0=g("oh1")[:], scalar=-BIG, in1=g("Lm")[:],
                                                         op0=ALU.mult, op1=ALU.add), r=[B_("oh1"), B_("Lm")], w=[B_("Lm2")])
            S.op("dve", lambda e: e.tensor_reduce(g("m2")[:], g("Lm2")[:], X_, ALU.max), r=[B_("Lm2")], w=[B_("m2")])
            S.op("dve", lambda e: e.tensor_tensor(g("oh2")[:], g("Lm2")[:], bc(g("m2")[:], 32), ALU.is_ge), r=[B_("Lm2"), B_("m2")], w=[B_("oh2")])
            S.op("dve", lambda e: e.tensor_tensor(g("dd")[:], g("m1")[:], g("m2")[:], ALU.subtract), r=[B_("m1"), B_("m2")], w=[B_("dd")])
            S.op("act", lambda e: e.activation(g("dd")[:], g("dd")[:], AF.Exp, scale=-1.0), r=[B_("dd")], w=[B_("dd")])
            S.op("dve", lambda e: e.tensor_scalar_add(g("dd")[:], g("dd")[:], 1.0), r=[B_("dd")], w=[B_("dd")])
            S.op("dve", lambda e: e.reciprocal(g("dd")[:], g("dd")[:]), r=[B_("dd")], w=[B_("dd")])
            S.op("dve", lambda e: e.tensor_tensor(g("w1")[:], g("dd")[:], g("gw")[:], ALU.mult), r=[B_("dd"), B_("gw")], w=[B_("w1")])
            S.op("dve", lambda e: e.tensor_tensor(g("w2")[:], g("gw")[:], g("w1")[:], ALU.subtract), r=[B_("gw"), B_("w1")], w=[B_("w2")])
            S.op("dve", lambda e: e.tensor_tensor(combw[:], g("oh1")[:], bc(g("w1")[:], 32), ALU.mult), r=[B_("oh1"), B_("w1")], w=[Bcombw])
            S.op("dve", lambda e: e.tensor_tensor(g("oh2")[:], g("oh2")[:], bc(g("w2")[:], 32), ALU.mult), r=[B_("oh2"), B_("w2")], w=[B_("oh2")])
            S.op("dve", lambda e: e.tensor_tensor(combw[:], combw[:], g("oh2")[:], ALU.add), r=[B_("oh2"), Bcombw], w=[Bcombw])
        Ycm.__exit__(None, None, None)
        if stage == "p3d":
            dbg_dump(combw[:].rearrange("p a b -> p (a b)"), Bcombw, 128, 512, 2048)
            S.wait_all("sp", list(all_bufs))
            return nc

        with scope() as p4:
            acc, Bacc = sb("acc", [128, NT, D], stack=p4)
            EW = [dict(wg=sb("ewg%d" % i, [128, 8, FF], BF16, stack=p4), wu=sb("ewu%d" % i, [128, 8, FF], BF16, stack=p4),
                       wd=sb("ewd%d" % i, [128, 4, D], BF16, stack=p4)) for i in range(2)]
            ACTT = [sb("actT%d" % i, [128, 4, 512], BF16, stack=p4) for i in range(2)]
            SGM = [sb("sgm%d" % i, [128, 512], stack=p4) for i in range(2)]
            l2g, Bl2g = sb("l2g", [128, D], stack=p4)
            l2b, Bl2b = sb("l2b", [128, D], stack=p4)
            S.dma("sp", l2g[:], ln2g.partition_broadcast(128), Bl2g, w=[Bl2g])
            S.dma("sp", l2b[:], ln2b.partition_broadcast(128), Bl2b, w=[Bl2b])
            H1 = [sb("h1_%d" % i, [128, D], stack=p4) for i in range(2)]
            ST4 = [sb("st4_%d" % i, [128, 12], stack=p4) for i in range(2)]
            MV4 = [sb("mv4_%d" % i, [128, 2], stack=p4) for i in range(2)]
            RS4 = [sb("rs4_%d" % i, [128, 1], stack=p4) for i in range(2)]
            AT = [sb("at_%d" % i, [128, D], stack=p4) for i in range(2)]
            Bost = [Buf("ost0"), Buf("ost1")]
            all_bufs.extend(Bost)
            NM4 = [sb("nm4_%d" % i, [128, 1], stack=p4) for i in range(2)]
            def final_tile(tile):
                i2 = tile % 2
                ts_ = slice(tile * 128, (tile + 1) * 128)
                h1t, Bh1t = H1[i2]
                at_, Bat = AT[i2]
                S.dma("sp", h1t[:], h1_d[ts_, :], Bh1t, r=[dbuf_h1], w=[Bh1t])
                S.op("dve", lambda e, tile=tile, at_=at_: e.tensor_tensor(at_[:], acc[:, tile, :], gtrow[:, 1, :], ALU.mult),
                     r=[Bacc, Bgtrow], w=[Bat])
                S.op("dve", lambda e, h1t=h1t, at_=at_: e.scalar_tensor_tensor(
                    out=at_[:], in0=h1t[:], scalar=ALPHA, in1=at_[:], op0=ALU.mult, op1=ALU.add),
                    r=[Bh1t, Bat], w=[Bat])
                st_, Bst = ST4[i2]; mv, Bmv = MV4[i2]; rs, Brs = RS4[i2]; nm, Bnm = NM4[i2]
                ln_norm(at_, Bat, h1t, Bh1t, st_, Bst, mv, Bmv, rs, Brs, nm, Bnm)
                S.op("dve", lambda e, h1t=h1t: e.tensor_tensor(h1t[:], h1t[:], l2g[:], ALU.mult), r=[Bh1t, Bl2g], w=[Bh1t])
                S.op("dve", lambda e, h1t=h1t: e.tensor_tensor(h1t[:], h1t[:], l2b[:], ALU.add), r=[Bh1t, Bl2b], w=[Bh1t])
                S.dma("pool", out_d[ts_, :], h1t[:], Bost[i2], r=[Bh1t])
            NEX = int(os.environ.get("NEXP", NE))
            it = 0
            iyc = [0]
            pend_dn = []
            for ex in range(NEX):
                ewg, Bewg = EW[ex % 2]["wg"]; ewu, Bewu = EW[ex % 2]["wu"]; ewd, Bewd = EW[ex % 2]["wd"]
                S.dma("pool", ewg[:], w_eg[ex].rearrange("(k p) c -> p k c", p=128), Bewg, w=[Bewg])
                S.dma("pool", ewu[:], w_eu[ex].rearrange("(k p) c -> p k c", p=128), Bewu, w=[Bewu])
                S.dma("pool", ewd[:], w_ed[ex].rearrange("(k p) c -> p k c", p=128), Bewd, w=[Bewd])
                for blk in range(4):
                    bs = slice(blk * 512, (blk + 1) * 512)
                    actT, BactT = ACTT[blk % 2]
                    for fc in range(4):
                        pg, Bpg = PS[(it % 2) * 2]
                        pu, Bpu = PS[(it % 2) * 2 + 1]
                        sgm, Bsgm = SGM[it % 2]
                        it += 1
                        for k in range(8):
                            mm(pg[:], ewg[:, k, fc * 128:(fc + 1) * 128], tTb[:, k, bs], k == 0, k == 7, [Bewg, BtTb], [Bpg])
                        for k in range(8):
                            mm(pu[:], ewu[:, k, fc * 128:(fc + 1) * 128], tTb[:, k, bs], k == 0, k == 7, [Bewu, BtTb], [Bpu])
                        S.op("act", lambda e, sgm=sgm, pg=pg: e.activation(sgm[:], pg[:], AF.Silu), r=[Bpg], w=[Bsgm])
                        S.op("dve", lambda e, actT=actT, fc=fc, sgm=sgm, pu=pu: e.tensor_tensor(actT[:, fc, :], sgm[:], pu[:], ALU.mult),
                             r=[Bsgm, Bpu], w=[BactT])

                    def down(ex=ex, blk=blk, actT=actT, BactT=BactT, ewd=ewd, Bewd=Bewd):
                        for tl in range(4):
                            tile = blk * 4 + tl
                            for half in range(2):
                                py, Bpy = PS[4 + iyc[0] % 2]
                                iyc[0] += 1
                                for fc in range(4):
                                    mm(py[:], actT[:, fc, tl * 128:(tl + 1) * 128], ewd[:, fc, half * 512:(half + 1) * 512],
                                       fc == 0, fc == 3, [BactT, Bewd], [Bpy])
                                a_ap = acc[:, tile, half * 512:(half + 1) * 512]
                                w_ap = combw[:, tile, ex:ex + 1]
                                if ex == 0:
                                    S.op("dve", lambda e, a_ap=a_ap, py=py, w_ap=w_ap: e.tensor_scalar(a_ap, py[:], w_ap, None, ALU.mult),
                                         r=[Bpy, Bcombw], w=[Bacc])
                                else:
                                    S.op("dve", lambda e, a_ap=a_ap, py=py, w_ap=w_ap: e.scalar_tensor_tensor(
                                        out=a_ap, in0=py[:], scalar=w_ap, in1=a_ap, op0=ALU.mult, op1=ALU.add),
                                        r=[Bpy, Bcombw, Bacc], w=[Bacc])
                            if ex == NEX - 1:
                                final_tile(tile)
                    if pend_dn:
                        pend_dn.pop(0)()
                    pend_dn.append(down)
            while pend_dn:
                pend_dn.pop(0)()
            S.wait_all("sp", list(all_bufs))
        return nc
    return nc


def _consts():
    p = np.arange(128, dtype=np.float32)
    j = p[:, None]
    i = p[None, :]
    cst = np.zeros((128, CW), np.float32)
    cst[:, C_ID:C_ID + 128] = np.eye(128, dtype=np.float32)
    cst[:, C_DF:C_DF + 128] = np.maximum(i - j, 0)
    cst[:, C_MF:C_MF + 128] = (i >= j)
    cst[:, C_DB:C_DB + 128] = np.maximum(j - i, 0)
    cst[:, C_MB:C_MB + 128] = (j >= i)
    cst[:, C_I1:C_I1 + 128] = i + 1
    cst[:, C_I2:C_I2 + 128] = 128 - i
    cst[:, C_PC] = 127 - p
    cst[:, C_PC + 1] = p
    cst[:, C_IE] = -(np.arange(128) % 32) / 32.0
    cb = np.zeros((128, CBW), np.float32)
    cb[:, CB_ID:CB_ID + 128] = np.eye(128)
    pr = np.zeros((128, 128), np.float32)
    for m in range(128):
        if m < 64:
            pr[m + 64, m] = -1.0
        else:
            pr[m - 64, m] = 1.0
    cb[:, CB_PR:CB_PR + 128] = pr
    cb[:, CB_ON:CB_ON + 128] = 1.0
    return cst, cb.astype(ml_dtypes.bfloat16)


def _angpos(tok_idx):
    tok_idx = np.asarray(tok_idx)
    row = (tok_idx // 64).astype(np.float32)
    col = (tok_idx % 64).astype(np.float32)
    sel_col = ((np.arange(128) % 64) >= 32)
    return np.where(sel_col[:, None], col[None, :], row[None, :]).astype(np.float32)


def prep_inputs(inp):
    f = lambda a: np.ascontiguousarray(np.asarray(a, dtype=np.float32))
    x = f(inp["x"]); ctx = f(inp["ctx"]); c = f(inp["c"]); c_ctx = f(inp["c_ctx"])
    cst0, cstb = _consts()
    b_in = f(inp["b_in"])[0]
    vecs = np.zeros((NVEC, 128), np.float32)
    vecs[R_BADA:R_BADA + 48] = f(inp["b_ada"])[0].reshape(48, 128)
    vecs[R_BIN:R_BIN + 80] = b_in.reshape(80, 128)
    vecs[R_CW:R_CW + 248] = f(inp["conv_dw"])[0].reshape(31 * 8, 128)
    vecs[R_CB:R_CB + 8] = f(inp["conv_dw_b"])[0].reshape(8, 128)
    vecs[R_LG:R_LG + 8] = f(inp["conv_ln_g"])[0].reshape(8, 128)
    vecs[R_LB:R_LB + 8] = f(inp["conv_ln_b"])[0].reshape(8, 128)
    vecs[R_BCO:R_BCO + 8] = f(inp["b_conv_out"])[0].reshape(8, 128)
    vecs[R_L1G:R_L1G + 8] = f(inp["ln1_g"])[0].reshape(8, 128)
    vecs[R_L1B:R_L1B + 8] = f(inp["ln1_b"])[0].reshape(8, 128)
    shared = {
        "cstb": cstb, "vecs": vecs,
        "w_ada": f(inp["w_ada"])[0], "b_ada": f(inp["b_ada"]), "w_in": f(inp["w_in"])[0], "b_in": f(inp["b_in"]),
        "decay": np.concatenate([f(inp["ret_decay_fwd"]), f(inp["ret_decay_bwd"])], axis=1),
        "w_ret_out": f(inp["w_ret_out"])[0], "w_conv_out": f(inp["w_conv_out"])[0],
        "w_mix_out": f(inp["w_mix_out"])[0], "b_mix_out": f(inp["b_mix_out"]),
        "ln1_g": f(inp["ln1_g"]), "ln1_b": f(inp["ln1_b"]), "ln2_g": f(inp["ln2_g"]), "ln2_b": f(inp["ln2_b"]),
        "w_router": np.ascontiguousarray(np.concatenate([f(inp["w_router_grp"])[0], f(inp["w_router_exp"])[0]], axis=1)),
        "b_router": np.concatenate([f(inp["b_router_grp"]), f(inp["b_router_exp"])], axis=1),
        "w_exp_gate": f(inp["w_exp_gate"])[0], "w_exp_up": f(inp["w_exp_up"])[0], "w_exp_down": f(inp["w_exp_down"])[0],
    }
    maps = []
    for core in range(8):
        b, s = core // 2, core % 2
        own0 = s * T
        oth0 = (1 - s) * T
        m = dict(shared)
        m["xo"] = np.ascontiguousarray(x[b, own0:own0 + T])
        m["xa"] = np.ascontiguousarray(np.concatenate([x[b, oth0:oth0 + T], ctx[b]], axis=0))
        halo = np.zeros((32, D), np.float32)
        hm = np.zeros(32, np.float32)
        if s == 1:
            halo[0:16] = x[b, own0 - 16:own0]; hm[0:16] = 1.0
        else:
            halo[16:32] = x[b, own0 + T:own0 + T + 16]; hm[16:32] = 1.0
        m["xh"] = halo
        m["cvec"] = np.ascontiguousarray(np.stack([c[b], c_ctx], axis=1))
        cst = cst0.copy()
        cst[:, C_HM:C_HM + 32] = hm[None, :]
        we = np.zeros((128, NTA, 4), np.float32)
        mo = np.arange(T, dtype=np.float32).reshape(16, 128).T
        mc = np.arange(256, dtype=np.float32).reshape(2, 128).T
        if s == 0:
            we[:, 0:16, 2] = mo; we[:, 0:16, 3] = 1.0
            we[:, 16:18, 0] = 255 - mc; we[:, 16:18, 1] = 1.0
            we[:, 16:18, 2] = mc + T; we[:, 16:18, 3] = 1.0
        else:
            we[:, 0:16, 0] = (T - 1) - mo; we[:, 0:16, 1] = 1.0
            we[:, 16:18, 0] = 255 - mc + T; we[:, 16:18, 1] = 1.0
            we[:, 16:18, 2] = mc; we[:, 16:18, 3] = 1.0
        cst[:, C_WE:C_WE + 72] = we.reshape(128, 72)
        m["cst"] = cst
        m["ango"] = _angpos(np.arange(own0, own0 + T))
        anga = np.zeros((128, NAUX), np.float32)
        anga[:, 0:T] = _angpos(np.arange(oth0, oth0 + T))
        m["anga"] = anga
        maps.append(m)
    return maps


_NC_CACHE = {}


def kernel(**inputs):
    maps = prep_inputs(inputs)
    if "nc" not in _NC_CACHE:
        _NC_CACHE["nc"] = build_program()
    nc = _NC_CACHE["nc"]
    res = run_bass_kernel_spmd(nc, maps, core_ids=list(range(8)))
    out = np.zeros((NB, SEQ, D), np.float32)
    for core in range(8):
        b, s = core // 2, core % 2
        out[b, s * T:(s + 1) * T] = np.asarray(res.results[core]["out"], dtype=np.float32)
    return out
```

```python
import contextlib
import math
import os
import numpy as np
import ml_dtypes
import concourse.bass as bass
import concourse.mybir as mybir
from concourse.bass_utils import run_bass_kernel_spmd

F32 = mybir.dt.float32
BF16 = mybir.dt.bfloat16
AF = mybir.ActivationFunctionType
ALU = mybir.AluOpType

D = 1024
SEQ = 4096
NB = 4
T = 2048
NT = 16
NAUX = 2304
NTA = 18
H = 8
DK = 128
DV = 256
Q_OFF, K_OFF, V_OFF, G_OFF, GLU_OFF, GATE_OFF = 0, 1024, 2048, 4096, 6144, 8192
NE = 32
FF = 512
EPS = 1e-5
ALPHA = 2.0 ** 0.25
KSCALE = 128.0 ** -0.5
PI = math.pi

C_ID = 0
C_DF = 128
C_MF = 256
C_DB = 384
C_MB = 512
C_I1 = 640
C_I2 = 768
C_PC = 896
C_IE = 898
C_WE = 900
C_HM = 972
CW = 1004
CB_ID, CB_PR, CB_ON, CBW = 0, 128, 256, 384
R_BADA = 0
R_BIN = 48
R_CW = 128
R_CB = 376
R_LG = 384
R_LB = 392
R_BCO = 400
R_L1G = 408
R_L1B = 416
NVEC = 512


class Buf:
    __slots__ = ("name", "w", "r", "sem", "cnt", "excl")

    def __init__(self, name, excl=False):
        self.name = name
        self.excl = excl
        self.w = None
        self.r = []
        self.sem = None
        self.cnt = 0


class Sched:
    def __init__(self, nc, es):
        self.nc = nc
        self.es = es
        self.eng = {"pe": nc.tensor, "act": nc.scalar, "dve": nc.vector, "pool": nc.gpsimd, "sp": nc.sync}
        self.sem = {}
        self.cnt = {}
        for k in self.eng:
            self.sem[k] = es.enter_context(nc.semaphore("s_" + k))
            self.cnt[k] = 0
        self.seen = {k: {} for k in self.eng}
        self.dsem = {}
        self.nd = 0

    def _waits(self, e, evs):
        need = {}
        for ev in evs:
            if ev is None:
                continue
            k, v = ev
            if k == e and e == "pe":
                continue
            if v > need.get(k, 0):
                need[k] = v
        for k, v in need.items():
            if self.seen[e].get(k, 0) >= v:
                continue
            self.seen[e][k] = v
            s = self.sem[k] if k in self.sem else self.dsem[k]
            self.eng[e].wait_ge(s, v)

    def _deps(self, r, w):
        evs = []
        for b in r:
            evs.append(b.w)
            if b.excl:
                evs.extend(b.r)
        for b in w:
            evs.append(b.w)
            evs.extend(b.r)
        return evs

    def op(self, e, fn, r=(), w=()):
        self._waits(e, self._deps(r, w))
        ins = fn(self.eng[e])
        self.cnt[e] += 1
        ins.then_inc(self.sem[e], 1)
        ev = (e, self.cnt[e])
        for b in r:
            b.r.append(ev)
        for b in w:
            b.w = ev
            b.r = []
        return ins

    def dma(self, q, out, in_, owner, r=(), w=(), join=False):
        if owner.sem is None:
            self.nd += 1
            owner.sem = "d%d" % self.nd
            self.dsem[owner.sem] = self.es.enter_context(self.nc.semaphore("sd%d" % self.nd))
        evs = []
        if not join and owner.cnt > 0:
            evs.append((owner.sem, owner.cnt))
        for b in r:
            evs.append(b.w)
        for b in w:
            if not (join and b.w is not None and b.w[0] == owner.sem):
                evs.append(b.w)
            evs.extend(b.r)
        self._waits(q, evs)
        ins = self.eng[q].dma_start(out=out, in_=in_)
        owner.cnt += 16
        ins.then_inc(self.dsem[owner.sem], 16)
        ev = (owner.sem, owner.cnt)
        for b in r:
            b.r.append(ev)
        for b in w:
            b.w = ev
            b.r = []
        return ins

    def wait_all(self, e, bufs):
        evs = []
        for b in bufs:
            evs.append(b.w)
            evs.extend(b.r)
        self._waits(e, evs)


def build_program(dbg=None, stage=None):
    nc = bass.Bass("TRN2", target_bir_lowering=False)

    def din(name, shape, dt=F32):
        return nc.dram_tensor(name, list(shape), dt, kind="ExternalInput").ap()

    xo = din("xo", [T, D])
    xa = din("xa", [NAUX, D])
    xh = din("xh", [32, D])
    cvec = din("cvec", [D, 2])
    cst_d = din("cst", [128, CW])
    cstb_d = din("cstb", [128, CBW], BF16)
    vecs_d = din("vecs", [NVEC, 128])
    ango_d = din("ango", [128, T])
    anga_d = din("anga", [128, NAUX])
    w_ada = din("w_ada", [D, 6 * D])
    b_ada = din("b_ada", [1, 6 * D])
    w_in = din("w_in", [D, 10240])
    b_in = din("b_in", [1, 10240])
    decay = din("decay", [1, 16])
    w_ro = din("w_ret_out", [2048, D])
    w_co = din("w_conv_out", [D, D])
    w_mo = din("w_mix_out", [D, D])
    b_mo = din("b_mix_out", [1, D])
    ln1g = din("ln1_g", [1, D])
    ln1b = din("ln1_b", [1, D])
    ln2g = din("ln2_g", [1, D])
    ln2b = din("ln2_b", [1, D])
    w_rt = din("w_router", [D, 36])
    b_rt = din("b_router", [1, 36])
    w_eg = din("w_exp_gate", [NE, D, FF])
    w_eu = din("w_exp_up", [NE, D, FF])
    w_ed = din("w_exp_down", [NE, FF, D])
    out_d = nc.dram_tensor("out", [T, D], F32, kind="ExternalOutput").ap()
    if dbg is not None:
        dbg_d = nc.dram_tensor("dbg", list(dbg), F32, kind="ExternalOutput").ap()
    uaux_d = nc.dram_tensor("uaux_s", [128, 8, NAUX], BF16).ap()
    sina_d = nc.dram_tensor("sina_s", [128, NAUX], F32).ap()
    cosa_d = nc.dram_tensor("cosa_s", [128, NAUX], F32).ap()
    retT_d = nc.dram_tensor("retT_s", [128, 16, T], BF16).ap()
    h1_d = nc.dram_tensor("h1_s", [T, D], F32).ap()

    es = contextlib.ExitStack()
    with es:
        S = Sched(nc, es)

        all_bufs = []
        fence = []
        scoped = {}

        def sb(name, shape, dt=F32, stack=es):
            t = stack.enter_context(nc.sbuf_tensor("sb_" + name, list(shape), dt))
            b = Buf(name)
            all_bufs.append(b)
            b.r = list(fence)
            scoped.setdefault(id(stack), []).append(b)
            return t, b

        @contextlib.contextmanager
        def scope():
            st = contextlib.ExitStack()
            with st:
                yield st
                evs = {}
                for b in scoped.pop(id(st), []):
                    for ev in ([b.w] if b.w else []) + b.r:
                        if ev[1] > evs.get(ev[0], 0):
                            evs[ev[0]] = ev[1]
                for k, v in evs.items():
                    fence.append((k, v))

        def ps(name, shape, dt=F32, stack=es):
            t = stack.enter_context(nc.psum_tensor("pp_" + name, list(shape), dt))
            b = Buf(name, excl=True)
            all_bufs.append(b)
            return t, b

        cst, Bcst = sb("cst", [128, CW])
        cstb, Bcstb = sb("cstb", [128, CBW], BF16)
        fm, Bfm = sb("fm", [128, NVEC])
        modfm, Bmod = sb("modfm", [128, 48, 2])
        sc1m, Bsc1m = sb("sc1m", [128, 8, 2])
        modB, BmodB = sb("modB", [128, 24, 2])
        cv, Bcv = sb("cv", [128, 8, 2])
        sv, Bsv = sb("sv", [128, 8, 2], BF16)
        svr, Bsvr = sb("svr", [128, 8, 128], BF16)
        sc1f, Bsc1f = sb("sc1f", [128, 8])
        lg, Blg = sb("lg", [128, 16])
        cdec, Bcdec = sb("cdec", [128, 16])
        kdec, Bkdec = sb("kdec", [128, 16])
        waux, Bwaux = sb("waux", [128, NTA, 16])
        gtrow, Bgtrow = sb("gtrow", [128, 2, D])
        negpi, Bnegpi = sb("negpi", [128, 1])
        epsb, Bepsb = sb("epsb", [128, 1])
        combw, Bcombw = sb("combw", [128, NT, NE])
        dbuf = Buf("dram_misc")
        dbuf_ret = Buf("dram_ret")
        daux = [Buf("dram_aux%d" % i) for i in range(NTA + 20)]
        all_bufs.extend(daux)
        daux_n = [0]
        dbuf_h1 = Buf("dram_h1")
        all_bufs.extend([dbuf, dbuf_ret, dbuf_h1])

        S.dma("sp", cst[:], cst_d, Bcst, w=[Bcst])
        S.dma("sp", cstb[:], cstb_d, Bcstb, w=[Bcstb])
        S.op("dve", lambda e: e.memset(negpi[:], -PI), w=[Bnegpi])
        S.op("dve", lambda e: e.memset(epsb[:], EPS), w=[Bepsb])
        ident = cst[:, C_ID:C_ID + 128]
        identb = cstb[:, CB_ID:CB_ID + 128]
        protb = cstb[:, CB_PR:CB_PR + 128]
        onesb = cstb[:, CB_ON:CB_ON + 128]

        PS = [ps("ps%d" % i, [128, 512]) for i in range(6)]
        PSBs = [ps("psb%d" % i, [128, 1024], BF16) for i in range(2)]

        def dbg_dump(tile_ap, buf, rows, cols, col0=0):
            S.dma("sp", dbg_d[0:rows, col0:col0 + cols], tile_ap, buf, r=[buf])

        with scope() as p0:
            vt_, Bvt = sb("vt", [128, 4, 128], stack=p0)
            S.dma("sp", vt_[:], vecs_d.rearrange("(a p) c -> p a c", p=128), Bvt, w=[Bvt])
            pT, BpT = PS[0]
            for a in range(4):
                S.op("pe", lambda e, a=a: e.transpose(pT[:, a * 128:(a + 1) * 128], vt_[:, a, :], ident),
                     r=[Bvt, Bcst], w=[BpT])
            S.op("dve", lambda e: e.tensor_copy(fm[:], pT[:]), r=[BpT], w=[Bfm])

            S.dma("sp", cv[:], cvec.rearrange("(k p) c -> p k c", p=128), Bcv, w=[Bcv])
            S.op("act", lambda e: e.activation(sv[:], cv[:], AF.Silu), r=[Bcv], w=[Bsv])
            for k in range(8):
                S.op("dve", lambda e, k=k: e.tensor_copy(svr[:, k, :], sv[:, k, 0:1].to_broadcast([128, 128])),
                     r=[Bsv], w=[Bsvr])
            barow, Bbarow = sb("barow", [128, 2, D], stack=p0)
            for i, c0 in enumerate((2 * D, 5 * D)):
                S.dma("sp", barow[:, i, :], b_ada[:, c0:c0 + D].partition_broadcast(128), Bbarow, w=[Bbarow],
                      join=True)
            wad, Bwad = sb("wad", [128, 8, 1536], BF16, stack=p0)
            pM, BpM = PS[1]

            def ada_piece(piece, wad, Bwad, pM, BpM, jbase, pRs, barow, Bbarow):
                for k in range(8):
                    S.dma("pool", wad[:, k, :], w_ada[k * 128:(k + 1) * 128, piece * 1536:(piece + 1) * 1536],
                          Bwad, w=[Bwad], join=(k > 0))
                for j in range(12):
                    jj = piece * 12 + j - jbase
                    for k in range(8):
                        S.op("pe", lambda e, j=j, jj=jj, k=k: e.matmul(
                            pM[:, jj * 2:jj * 2 + 2], lhsT=wad[:, k, j * 128:(j + 1) * 128], rhs=sv[:, k, :],
                            start=(k == 0), stop=(k == 7)), r=[Bwad, Bsv], w=[BpM])
                for gi, c0 in enumerate((2 * D, 5 * D)):
                    for hf in range(2):
                        cc = c0 + hf * 512
                        if cc // 1536 != piece and (cc + 511) // 1536 != piece:
                            continue
                        lo = max(cc, piece * 1536)
                        hi = min(cc + 512, (piece + 1) * 1536)
                        pR, BpR = pRs[gi]
                        for k in range(8):
                            S.op("pe", lambda e, k=k, lo=lo, hi=hi, pR=pR: e.matmul(
                                pR[:, 0:hi - lo], lhsT=svr[:, k, :],
                                rhs=wad[:, k, lo - piece * 1536:hi - piece * 1536],
                                start=(k == 0), stop=(k == 7)), r=[Bwad, Bsvr], w=[BpR])
                        S.op("dve", lambda e, lo=lo, hi=hi, pR=pR, gi=gi, c0=c0: e.tensor_tensor(
                            gtrow[:, gi, lo - c0:hi - c0], pR[:, 0:hi - lo], barow[:, gi, lo - c0:hi - c0], ALU.add),
                            r=[BpR, Bbarow], w=[Bgtrow])

            for piece in range(2):
                ada_piece(piece, wad, Bwad, pM, BpM, 0, [PS[2], PS[3]], barow, Bbarow)
            S.op("dve", lambda e: e.tensor_tensor(
                modfm[:, 0:24, :], pM[:, 0:48].rearrange("p (j c) -> p j c", c=2),
                fm[:, R_BADA:R_BADA + 24].unsqueeze(2).to_broadcast([128, 24, 2]), ALU.add),
                r=[BpM, Bfm], w=[Bmod])
            S.op("dve", lambda e: e.tensor_scalar_add(sc1m[:], modfm[:, 8:16, :], 1.0), r=[Bmod], w=[Bsc1m])

            dc, Bdc = sb("dc", [128, 16], stack=p0)
            S.dma("sp", dc[:], decay.partition_broadcast(128), Bdc, w=[Bdc])
            S.op("act", lambda e: e.activation(dc[:], dc[:], AF.Exp, scale=-1.0), r=[Bdc], w=[Bdc])
            S.op("dve", lambda e: e.tensor_scalar_add(dc[:], dc[:], 1.0), r=[Bdc], w=[Bdc])
            S.op("act", lambda e: e.activation(lg[:], dc[:], AF.Ln), r=[Bdc], w=[Blg])
            S.op("dve", lambda e: e.tensor_scalar_mul(lg[:], lg[:], -1.0), r=[Blg], w=[Blg])
            S.op("act", lambda e: e.activation(cdec[:], lg[:], AF.Exp, scale=128.0), r=[Blg], w=[Bcdec])
            S.op("act", lambda e: e.activation(kdec[:, 0:8], lg[:, 0:8], AF.Exp, scale=cst[:, C_PC:C_PC + 1]),
                 r=[Blg, Bcst], w=[Bkdec])
            S.op("act", lambda e: e.activation(kdec[:, 8:16], lg[:, 8:16], AF.Exp, scale=cst[:, C_PC + 1:C_PC + 2]),
                 r=[Blg, Bcst], w=[Bkdec])
            S.op("dve", lambda e: e.tensor_scalar_mul(kdec[:], kdec[:], KSCALE), r=[Bkdec], w=[Bkdec])
            for t in range(NTA):
                for d_ in range(2):
                    c0 = C_WE + t * 4 + d_ * 2
                    S.op("act", lambda e, t=t, d_=d_, c0=c0: e.activation(
                        waux[:, t, d_ * 8:(d_ + 1) * 8], lg[:, d_ * 8:(d_ + 1) * 8], AF.Exp,
                        scale=cst[:, c0:c0 + 1]), r=[Blg, Bcst], w=[Bwaux])
                    S.op("dve", lambda e, t=t, d_=d_, c0=c0: e.tensor_scalar(
                        waux[:, t, d_ * 8:(d_ + 1) * 8], waux[:, t, d_ * 8:(d_ + 1) * 8],
                        cst[:, c0 + 1:c0 + 2], KSCALE, ALU.mult, ALU.mult), r=[Bwaux, Bcst], w=[Bwaux])

        if stage == "p0":
            dbg_dump(gtrow[:, 0, :], Bgtrow, 128, 1024, 0)
            dbg_dump(gtrow[:, 1, :], Bgtrow, 128, 1024, 1024)
            dbg_dump(modfm[:].rearrange("p a b -> p (a b)"), Bmod, 128, 96, 2048)
            dbg_dump(waux[:].rearrange("p a b -> p (a b)"), Bwaux, 128, 288, 2144)
            dbg_dump(kdec[:], Bkdec, 128, 16, 2432)
            dbg_dump(cdec[:], Bcdec, 128, 16, 2448)
            dbg_dump(lg[:], Blg, 128, 16, 2464)
            dbg_dump(fm[:], Bfm, 128, 512, 2480)
            S.wait_all("sp", [Bgtrow, Bmod, Bwaux, Bkdec, Bcdec, Blg, Bfm])
            return nc

        def mm(out, lhsT, rhs, start, stop, r, w):
            return S.op("pe", lambda e: e.matmul(out, lhsT=lhsT, rhs=rhs, start=start, stop=stop), r=r, w=w)

        u_own, Bu = sb("u_own", [128, 8, T], BF16)
        u_halo, Buh = sb("u_halo", [128, 8, 32], BF16)
        p12cm = scope()
        p12 = p12cm.__enter__()
        sino, Bsino = sb("sino", [128, T], stack=p12)
        coso, Bcoso = sb("coso", [128, T], stack=p12)
        inv, Binv = sb("inv", [128, 1], stack=p12)
        S.op("act", lambda e: e.activation(inv[:], cst[:, C_IE:C_IE + 1], AF.Exp, scale=math.log(10000.0)),
             r=[Bcst], w=[Binv])
        W = [dict(wq=sb("wq%d" % i, [128, 8, 128], BF16, stack=p12), wk=sb("wk%d" % i, [128, 8, 128], BF16, stack=p12),
                  wvg=sb("wvg%d" % i, [128, 8, 512], BF16, stack=p12), bvg=sb("bvgb%d" % i, [1, 512], BF16, stack=p12))
             for i in range(2)]

        def load_head_w(h):
            Wd = W[h % 2]
            wq, Bwq = Wd["wq"]; wk, Bwk = Wd["wk"]; wvg, Bwvg = Wd["wvg"]; bvg, Bbvg = Wd["bvg"]
            S.dma("pool", wq[:], w_in[:, Q_OFF + h * 128:Q_OFF + (h + 1) * 128].rearrange("(k p) c -> p k c", p=128),
                  Bwq, w=[Bwq])
            S.dma("pool", wk[:], w_in[:, K_OFF + h * 128:K_OFF + (h + 1) * 128].rearrange("(k p) c -> p k c", p=128),
                  Bwk, w=[Bwk])
            S.dma("pool", wvg[:, :, 0:256],
                  w_in[:, V_OFF + h * 256:V_OFF + (h + 1) * 256].rearrange("(k p) c -> p k c", p=128), Bwvg, w=[Bwvg])
            S.dma("pool", wvg[:, :, 256:512],
                  w_in[:, G_OFF + h * 256:G_OFF + (h + 1) * 256].rearrange("(k p) c -> p k c", p=128), Bwvg,
                  w=[Bwvg], join=True)
            S.dma("pool", bvg[:, 0:256], b_in[:, V_OFF + h * 256:V_OFF + (h + 1) * 256], Bbvg, w=[Bbvg])
            S.dma("pool", bvg[:, 256:512], b_in[:, G_OFF + h * 256:G_OFF + (h + 1) * 256], Bbvg, w=[Bbvg], join=True)

        load_head_w(0)
        with scope() as p1:
            xt = [sb("xt%d" % i, [128, D], stack=p1) for i in range(4)]
            ust = [sb("ust%d" % i, [128, 8, 128], BF16, stack=p1) for i in range(2)]
            utmp = [sb("utmp%d" % i, [128, 4, 128], stack=p1) for i in range(2)]
            ang, Bang = sb("ang", [128, 512], stack=p1)
            a1, Ba1 = sb("a1", [128, 512], stack=p1)
            a2, Ba2 = sb("a2", [128, 512], stack=p1)
            ai, Bai = sb("ai", [128, 512], mybir.dt.int32, stack=p1)
            so, Bso = sb("so", [128, 512], stack=p1)
            co, Bco = sb("co", [128, 512], stack=p1)
            inv2, Binv2 = sb("inv2", [128, 1], stack=p1)
            S.op("dve", lambda e: e.tensor_scalar_mul(inv2[:], inv[:], 1.0 / (2 * PI)), r=[Binv], w=[Binv2])
            blocks = [("own", i * 512, 512) for i in range(4)] + [("aux", i * 512, 512) for i in range(4)] + \
                     [("aux", 2048, 256)]
            SC = 2 * PI * (1.0 - 1e-7)

            def sin_of(out_ap, Bout, n, shift):
                S.op("dve", lambda e: e.tensor_scalar(a1[:, 0:n], ang[:, 0:n], inv2[:, 0:1], shift, ALU.mult, ALU.add),
                     r=[Bang, Binv2], w=[Ba1])
                S.op("dve", lambda e: e.tensor_copy(ai[:, 0:n], a1[:, 0:n]), r=[Ba1], w=[Bai])
                S.op("dve", lambda e: e.tensor_copy(a2[:, 0:n], ai[:, 0:n]), r=[Bai], w=[Ba2])
                S.op("dve", lambda e: e.tensor_tensor(a1[:, 0:n], a1[:, 0:n], a2[:, 0:n], ALU.subtract),
                     r=[Ba1, Ba2], w=[Ba1])
                S.op("dve", lambda e: e.tensor_single_scalar(a2[:, 0:n], a1[:, 0:n], 0.5, ALU.is_gt),
                     r=[Ba1], w=[Ba2])
                S.op("dve", lambda e: e.tensor_tensor(a1[:, 0:n], a1[:, 0:n], a2[:, 0:n], ALU.subtract),
                     r=[Ba1, Ba2], w=[Ba1])
                S.op("act", lambda e: e.activation(out_ap, a1[:, 0:n], AF.Sin, scale=SC), r=[Ba1], w=[Bout])

            def rope_block(kind, c0, n):
                src = ango_d if kind == "own" else anga_d
                S.dma("sp", ang[:, 0:n], src[:, c0:c0 + n], Bang, w=[Bang])
                s_out, Bs_out = (sino[:, c0:c0 + n], Bsino) if kind == "own" else (so[:, 0:n], Bso)
                c_out, Bc_out = (coso[:, c0:c0 + n], Bcoso) if kind == "own" else (co[:, 0:n], Bco)
                sin_of(s_out, Bs_out, n, 0.0)
                sin_of(c_out, Bc_out, n, 0.25)
                if kind == "aux":
                    S.dma("act", sina_d[:, c0:c0 + n], so[:, 0:n], Bso, r=[Bso], w=[daux[NTA + daux_n[0]]])
                    S.dma("act", cosa_d[:, c0:c0 + n], co[:, 0:n], Bco, r=[Bco], w=[daux[NTA + 10 + daux_n[0]]])
                    daux_n[0] += 1

            rope_todo = list(blocks)
            wad2, Bwad2 = sb("wad2", [128, 8, 1536], BF16, stack=p1)
            barow2, Bbarow2 = sb("barow2", [128, 2, D], stack=p1)
            S.dma("sp", barow2[:, 1, :], b_ada[:, 5 * D:6 * D].partition_broadcast(128), Bbarow2, w=[Bbarow2])

            def ada_late(piece):
                ada_piece(piece, wad2, Bwad2, PS[4][0], PS[4][1], 24, [PS[5], PS[5]], barow2, Bbarow2)
                if piece == 3:
                    S.op("dve", lambda e: e.tensor_tensor(
                        modB[:], PS[4][0][:, 0:48].rearrange("p (j c) -> p j c", c=2),
                        fm[:, R_BADA + 24:R_BADA + 48].unsqueeze(2).to_broadcast([128, 24, 2]), ALU.add),
                        r=[PS[4][1], Bfm], w=[BmodB])
                    S.op("dve", lambda e: e.tensor_scalar_add(sc1f[:], modB[:, 8:16, 0], 1.0), r=[BmodB], w=[Bsc1f])
            tiles = [("own", i) for i in range(NT)] + [("aux", i) for i in range(NTA)] + [("halo", 0)]
            for n, (kind, i) in enumerate(tiles):
                xtt, Bxt = xt[n % 4]
                rows = 32 if kind == "halo" else 128
                src = {"own": xo, "aux": xa, "halo": xh}[kind]
                r0 = 0 if kind == "halo" else i * 128
                S.dma("sp", xtt[0:rows, :], src[r0:r0 + rows, :], Bxt, w=[Bxt])
                which = 1 if (kind == "aux" and i >= 16) else 0
                pa, Bpa = PS[(n % 2) * 2]
                pb, Bpb = PS[(n % 2) * 2 + 1]
                for dt in range(8):
                    pp, Bpp = (pa, Bpa) if dt < 4 else (pb, Bpb)
                    S.op("pe", lambda e, dt=dt, pp=pp, rows=rows, xtt=xtt: e.transpose(
                        pp[:, (dt % 4) * 128:(dt % 4) * 128 + rows], xtt[0:rows, dt * 128:(dt + 1) * 128],
                        ident[0:rows, 0:rows]), r=[Bxt, Bcst], w=[Bpp])
                if kind == "aux":
                    dst, Bdst = ust[i % 2]
                if kind == "own":
                    o3, Bo = u_own[:, :, i * 128:(i + 1) * 128], Bu
                elif kind == "halo":
                    o3, Bo = u_halo[:, :, :], Buh
                else:
                    o3, Bo = dst[:, :, :], Bdst
                for dt in range(4):
                    S.op("act", lambda e, o3=o3, pa=pa, dt=dt, rows=rows, which=which: e.activation(
                        o3[:, dt, :], pa[:, dt * 128:dt * 128 + rows], AF.Identity,
                        bias=modfm[:, dt, which:which + 1], scale=sc1m[:, dt, which:which + 1]),
                        r=[Bpa, Bmod, Bsc1m], w=[Bo])
                tq, Btq = utmp[n % 2]
                pb3 = pb[:].rearrange("p (a b) -> p a b", a=4)[:, :, 0:rows]
                S.op("dve", lambda e, tq=tq, pb3=pb3, rows=rows, which=which: e.tensor_tensor(
                    tq[:, :, 0:rows], pb3, sc1m[:, 4:8, which:which + 1].to_broadcast([128, 4, rows]), ALU.mult),
                    r=[Bpb, Bsc1m], w=[Btq])
                S.op("dve", lambda e, tq=tq, o3=o3, rows=rows, which=which: e.tensor_tensor(
                    o3[:, 4:8, :], tq[:, :, 0:rows], modfm[:, 4:8, which:which + 1].to_broadcast([128, 4, rows]), ALU.add),
                    r=[Btq, Bmod], w=[Bo])
                if kind == "aux":
                    S.dma("act", uaux_d[:, :, i * 128:(i + 1) * 128], dst[:], Bdst, r=[Bdst], w=[daux[i]])
                if n % 4 == 1 and rope_todo:
                    rope_block(*rope_todo.pop(0))
                if n == 5:
                    ada_late(2)
                if n == 13:
                    ada_late(3)

            while rope_todo:
                rope_block(*rope_todo.pop(0))
        if stage == "p1":
            dbg_dump(sino[:], Bsino, 128, 2048, 0)
            dbg_dump(coso[:], Bcoso, 128, 2048, 2048)
            with scope() as pd:
                tmpf, Btmpf = sb("tmpf", [128, 4096], stack=pd)
                S.op("dve", lambda e: e.tensor_copy(tmpf[:, 0:2048], u_own[:, 0, :]), r=[Bu], w=[Btmpf])
                S.op("dve", lambda e: e.tensor_copy(tmpf[:, 2048:4096], u_own[:, 7, :]), r=[Bu], w=[Btmpf])
                dbg_dump(tmpf[:], Btmpf, 128, 4096, 4096)
                S.wait_all("sp", [Btmpf, Bsino, Bcoso])
                S.wait_all("sp", list(all_bufs))
            p12cm.__exit__(None, None, None)
            return nc

        with scope() as p2:
            def sb2(name, shape, dt=F32):
                return sb(name, shape, dt, stack=p2)
            qT, BqT = sb2("qT", [128, T], BF16)
            kT, BkT = sb2("kT", [128, T], BF16)
            ktf, Bktf = sb2("ktf", [128, NT, 128], BF16)
            ktb, Bktb = sb2("ktb", [128, NT, 128], BF16)
            vv, Bvv = sb2("vv", [128, NT, 256], BF16)
            sg, Bsg = sb2("sg", [128, NT, 256], BF16)
            Sf16, BSf16 = sb2("Sf16", [128, NT, 256], BF16)
            Sb16, BSb16 = sb2("Sb16", [128, NT, 256], BF16)
            retT, BretT = sb2("retT", [128, 2, T], BF16)
            maskT, BmaskT = sb2("maskT", [128, 128])
            qdf, Bqdf = sb2("qdf", [128, 128])
            qdb, Bqdb = sb2("qdb", [128, 128])
            m1, Bm1 = sb2("m1", [128, 128])
            m2, Bm2 = sb2("m2", [128, 128])
            UA = [sb2("ua%d" % i, [128, 8, 512], BF16) for i in range(2)]
            SA = [sb2("sa%d" % i, [128, 512]) for i in range(2)]
            CA = [sb2("ca%d" % i, [128, 512]) for i in range(2)]
            KTA = [sb2("kta%d" % i, [128, 4, 128], BF16) for i in range(2)]
            VA = [sb2("va%d" % i, [128, 2, 512], BF16) for i in range(2)]
            QB = [sb2("qb%d" % i, [128, 512], BF16) for i in range(2)]
            T1 = [sb2("t1_%d" % i, [128, 512]) for i in range(2)]
            T2 = [sb2("t2_%d" % i, [128, 512]) for i in range(2)]
            Sst = [sb2("Sst%d" % i, [128, 256]) for i in range(2)]
            OT = [sb2("ot%d" % i, [128, 256]) for i in range(2)]
            R16 = [sb2("r16_%d" % i, [128, 256], BF16) for i in range(2)]
            STT = [sb2("stt%d" % i, [128, 6]) for i in range(2)]
            MV = [sb2("mv%d" % i, [128, 2]) for i in range(2)]
            RS = [sb2("rs%d" % i, [128, 1]) for i in range(2)]
            SM = [sb2("sm%d" % i, [128, 128], BF16) for i in range(2)]
            QF = [sb2("qf%d" % i, [128, 128], BF16) for i in range(2)]
            QBk = [sb2("qbk%d" % i, [128, 128], BF16) for i in range(2)]

            ridx = [0]
            pend = []
            two_pr = [False]

            def rope_submit(wt, Bwt, rhs_fn, Brhs, n, bias_col, cos_ap, sin_ap, Btabs, out_ap, Bout):
                i = ridx[0] % 2
                ridx[0] += 1
                pq, Bpq = PS[i]
                pr, Bpr = PS[2 + i] if two_pr[0] else PS[2]
                qb, Bqb = QB[i]; t1, Bt1 = T1[i]; t2, Bt2 = T2[i]
                for k in range(8):
                    mm(pq[:, 0:n], wt[:, k, :], rhs_fn(k), k == 0, k == 7, [Bwt, Brhs], [Bpq])
                bias = fm[:, bias_col:bias_col + 1]
                S.op("act", lambda e: e.activation(qb[:, 0:n], pq[:, 0:n], AF.Identity, bias=bias),
                     r=[Bpq, Bfm], w=[Bqb])
                S.op("dve", lambda e: e.scalar_tensor_tensor(out=t1[:, 0:n], in0=pq[:, 0:n], scalar=bias, in1=cos_ap,
                                                             op0=ALU.add, op1=ALU.mult),
                     r=[Bpq, Bfm] + Btabs, w=[Bt1])

                def stage2():
                    mm(pr[:, 0:n], protb, qb[:, 0:n], True, True, [Bcstb, Bqb], [Bpr])
                    S.op("dve", lambda e: e.tensor_tensor(t2[:, 0:n], pr[:, 0:n], sin_ap, ALU.mult),
                         r=[Bpr] + Btabs, w=[Bt2])
                    S.op("dve", lambda e: e.tensor_tensor(out_ap, t1[:, 0:n], t2[:, 0:n], ALU.add),
                         r=[Bt1, Bt2], w=[Bout])
                pend.append(stage2)
                if len(pend) > 1:
                    pend.pop(0)()

            def rope_flush():
                while pend:
                    pend.pop(0)()

            KTAB = [sb2("kTa%d" % i, [128, 512], BF16) for i in range(2)]
            NH = int(os.environ.get('NHEADS', H))
            for h in range(NH):
                Wd = W[h % 2]
                wq, Bwq = Wd["wq"]; wk, Bwk = Wd["wk"]; wvg, Bwvg = Wd["wvg"]; bvg, Bbvg = Wd["bvg"]
                if h + 1 < NH:
                    load_head_w(h + 1)
                lgf = lg[:, h:h + 1]
                lgb = lg[:, 8 + h:9 + h]
                S.op("act", lambda e: e.activation(m1[:], cst[:, C_DF:C_DF + 128], AF.Exp, scale=lgf),
                     r=[Bcst, Blg], w=[Bm1])
                S.op("act", lambda e: e.activation(m2[:], cst[:, C_DB:C_DB + 128], AF.Exp, scale=lgb),
                     r=[Bcst, Blg], w=[Bm2])
                S.op("dve", lambda e: e.scalar_tensor_tensor(out=m1[:], in0=m1[:], scalar=KSCALE,
                                                             in1=cst[:, C_MF:C_MF + 128], op0=ALU.mult, op1=ALU.mult),
                     r=[Bm1, Bcst], w=[Bm1])
                S.op("dve", lambda e: e.scalar_tensor_tensor(out=m2[:], in0=m2[:], scalar=KSCALE,
                                                             in1=cst[:, C_MB:C_MB + 128], op0=ALU.mult, op1=ALU.mult),
                     r=[Bm2, Bcst], w=[Bm2])
                S.op("dve", lambda e: e.tensor_tensor(maskT[:], m1[:], m2[:], ALU.add), r=[Bm1, Bm2], w=[BmaskT])
                S.op("act", lambda e: e.activation(qdf[:], cst[:, C_I1:C_I1 + 128], AF.Exp, scale=lgf),
                     r=[Bcst, Blg], w=[Bqdf])
                S.op("act", lambda e: e.activation(qdb[:], cst[:, C_I2:C_I2 + 128], AF.Exp, scale=lgb),
                     r=[Bcst, Blg], w=[Bqdb])

                psfb, Bpsfb = PS[5]
                ablocks = [(i * 512, 512) for i in range(4)] + [(2048, 256)]

                def aux_load(bi):
                    c0, n = ablocks[bi]
                    ua, Bua = UA[bi % 2]; sa, Bsa = SA[bi % 2]; ca, Bca = CA[bi % 2]
                    S.dma("sp", ua[:, :, 0:n], uaux_d[:, :, c0:c0 + n], Bua, r=daux, w=[Bua])
                    S.dma("sp", sa[:, 0:n], sina_d[:, c0:c0 + n], Bsa, r=daux, w=[Bsa])
                    S.dma("sp", ca[:, 0:n], cosa_d[:, c0:c0 + n], Bca, r=daux, w=[Bca])

                def aux_rope(bi):
                    c0, n = ablocks[bi]
                    ua, Bua = UA[bi % 2]; sa, Bsa = SA[bi % 2]; ca, Bca = CA[bi % 2]
                    kTa, BkTa = KTAB[bi % 2]
                    rope_submit(wk, Bwk, lambda k, ua=ua, n=n: ua[:, k, 0:n], Bua, n, R_BIN + 8 + h,
                                ca[:, 0:n], sa[:, 0:n], [Bsa, Bca], kTa[:, 0:n], BkTa)

                two_pr[0] = False
                aux_load(0)
                aux_rope(0)
                stage2q = []
                ones1 = cstb[0:1, CB_ON:CB_ON + 128]
                pvi = [0]

                def aux_block(bi):
                    c0, n = ablocks[bi]
                    nt_ = n // 128
                    ua, Bua = UA[bi % 2]
                    kTa, BkTa = KTAB[bi % 2]
                    pb_, Bpb_ = PSBs[bi % 2]
                    kta, Bkta = KTA[bi % 2]
                    for pr2 in range(nt_ // 2):
                        pv, Bpv = PS[4] if pvi[0] % 2 == 0 else PS[3]
                        va, Bva = VA[pvi[0] % 2]
                        pvi[0] += 1
                        t0_ = c0 // 128 + pr2 * 2
                        for j in range(2):
                            tt = pr2 * 2 + j
                            for k in range(8):
                                mm(pv[:, j * 256:(j + 1) * 256], ua[:, k, tt * 128:(tt + 1) * 128], wvg[:, k, 0:256], k == 0, False,
                                   [Bua, Bwvg], [Bpv])
                            mm(pv[:, j * 256:(j + 1) * 256], ones1, bvg[0:1, 0:256], False, True, [Bcstb, Bbvg], [Bpv])
                        if pr2 == 0:
                            for tt in range(nt_):
                                S.op("pe", lambda e, tt=tt: e.transpose(pb_[:, tt * 128:(tt + 1) * 128], kTa[:, tt * 128:(tt + 1) * 128], identb),
                                     r=[BkTa, Bcstb], w=[Bpb_])
                            S.op("act", lambda e: e.activation(kta[:, 0:nt_, :], pb_[:, 0:n].rearrange("p (a b) -> p a b", b=128), AF.Copy),
                                 r=[Bpb_], w=[Bkta])
                        pv3 = pv[:].rearrange("p (a b) -> p a b", a=2)
                        S.op("dve", lambda e, va=va, pv3=pv3, t0_=t0_: e.tensor_tensor(
                            va[:, :, 0:256], pv3, waux[:, t0_:t0_ + 2, h:h + 1].to_broadcast([128, 2, 256]), ALU.mult),
                            r=[Bpv, Bwaux], w=[Bva])
                        S.op("dve", lambda e, va=va, pv3=pv3, t0_=t0_: e.tensor_tensor(
                            va[:, :, 256:512], pv3, waux[:, t0_:t0_ + 2, 8 + h:9 + h].to_broadcast([128, 2, 256]), ALU.mult),
                            r=[Bpv, Bwaux], w=[Bva])

                        def s2(va=va, Bva=Bva, t0_=t0_, pr2=pr2):
                            for j in range(2):
                                t = t0_ + j
                                mm(psfb[:], kta[:, pr2 * 2 + j, :], va[:, j, :], t == 0, t == NTA - 1, [Bkta, Bva], [Bpsfb])
                        stage2q.append(s2)
                        if len(stage2q) > 1:
                            stage2q.pop(0)()

                for bi, (c0, n) in enumerate(ablocks):
                    if bi + 1 < len(ablocks):
                        aux_load(bi + 1)
                    rope_flush()
                    aux_block(bi)
                    if bi + 1 < len(ablocks):
                        aux_rope(bi + 1)
                while stage2q:
                    stage2q.pop(0)()
                sf, Bsf = Sst[0]
                sbk, Bsbk = Sst[1]
                S.op("dve", lambda e: e.tensor_copy(sf[:], psfb[:, 0:256]), r=[Bpsfb], w=[Bsf])
                S.op("dve", lambda e: e.tensor_copy(sbk[:], psfb[:, 256:512]), r=[Bpsfb], w=[Bsbk])

                two_pr[0] = True
                for blk in range(4):
                    cs = slice(blk * 512, (blk + 1) * 512)
                    rope_submit(wk, Bwk, lambda k, cs=cs: u_own[:, k, cs], Bu, 512, R_BIN + 8 + h,
                                coso[:, cs], sino[:, cs], [Bsino, Bcoso], kT[:, cs], BkT)
                    rope_submit(wq, Bwq, lambda k, cs=cs: u_own[:, k, cs], Bu, 512, R_BIN + h,
                                coso[:, cs], sino[:, cs], [Bsino, Bcoso], qT[:, cs], BqT)
                rope_flush()
                for c in range(NT):
                    pv, Bpv = PS[c % 2 + 4]
                    for k in range(8):
                        mm(pv[:], u_own[:, k, c * 128:(c + 1) * 128], wvg[:, k, :], k == 0, False, [Bu, Bwvg], [Bpv])
                    mm(pv[:], ones1, bvg[0:1, :], False, True, [Bcstb, Bbvg], [Bpv])
                    pb_, Bpb_ = PSBs[c % 2]
                    S.op("pe", lambda e, c=c, pb_=pb_: e.transpose(
                        pb_[:, 0:128], kT[:, c * 128:(c + 1) * 128], identb), r=[BkT, Bcstb], w=[Bpb_])
                    S.op("act", lambda e, c=c, pb_=pb_: e.activation(
                        ktf[:, c, :], pb_[:, 0:128], AF.Copy, scale=kdec[:, h:h + 1]), r=[Bpb_, Bkdec], w=[Bktf])
                    S.op("act", lambda e, c=c, pb_=pb_: e.activation(
                        ktb[:, c, :], pb_[:, 0:128], AF.Copy, scale=kdec[:, 8 + h:9 + h]), r=[Bpb_, Bkdec], w=[Bktb])
                    S.op("dve", lambda e, c=c, pv=pv: e.tensor_copy(vv[:, c, :], pv[:, 0:256]), r=[Bpv], w=[Bvv])
                    S.op("act", lambda e, c=c, pv=pv: e.activation(sg[:, c, :], pv[:, 256:512], AF.Silu), r=[Bpv], w=[Bsg])
                S.op("act", lambda e: e.activation(Sf16[:, 0, :], sf[:], AF.Copy), r=[Bsf], w=[BSf16])
                S.op("act", lambda e: e.activation(Sb16[:, NT - 1, :], sbk[:], AF.Copy), r=[Bsbk], w=[BSb16])
                for c in range(NT - 1):
                    pd_, Bpd = PS[4 + c % 2]
                    mm(pd_[:, 0:256], ktf[:, c, :], vv[:, c, :], True, True, [Bktf, Bvv], [Bpd])
                    cb_ = NT - 1 - c
                    mm(pd_[:, 256:512], ktb[:, cb_, :], vv[:, cb_, :], True, True, [Bktb, Bvv], [Bpd])
                    S.op("dve", lambda e, pd_=pd_: e.scalar_tensor_tensor(
                        out=sf[:], in0=sf[:], scalar=cdec[:, h:h + 1], in1=pd_[:, 0:256], op0=ALU.mult, op1=ALU.add),
                        r=[Bsf, Bcdec, Bpd], w=[Bsf])
                    S.op("dve", lambda e, pd_=pd_: e.scalar_tensor_tensor(
                        out=sbk[:], in0=sbk[:], scalar=cdec[:, 8 + h:9 + h], in1=pd_[:, 256:512], op0=ALU.mult,
                        op1=ALU.add), r=[Bsbk, Bcdec, Bpd], w=[Bsbk])
                    S.op("act", lambda e, c=c: e.activation(Sf16[:, c + 1, :], sf[:], AF.Copy), r=[Bsf], w=[BSf16])
                    S.op("act", lambda e, cb_=cb_: e.activation(Sb16[:, cb_ - 1, :], sbk[:], AF.Copy),
                         r=[Bsbk], w=[BSb16])

                def sw_a(c):
                    cs = slice(c * 128, (c + 1) * 128)
                    i2 = c % 2
                    psc, Bpsc = PS[i2]
                    sm, Bsm = SM[i2]; qf, Bqf = QF[i2]; qbk, Bqbk = QBk[i2]
                    mm(psc[:, 0:128], kT[:, cs], qT[:, cs], True, True, [BkT, BqT], [Bpsc])
                    S.op("dve", lambda e: e.tensor_tensor(sm[:], psc[:, 0:128], maskT[:], ALU.mult),
                         r=[Bpsc, BmaskT], w=[Bsm])
                    S.op("pool", lambda e: e.tensor_tensor(qf[:], qT[:, cs], qdf[:], ALU.mult), r=[BqT, Bqdf], w=[Bqf])
                    S.op("pool", lambda e: e.tensor_tensor(qbk[:], qT[:, cs], qdb[:], ALU.mult), r=[BqT, Bqdb], w=[Bqbk])

                def sw_b(c):
                    i2 = c % 2
                    po, Bpo = PS[2 + i2]
                    sm, Bsm = SM[i2]; qf, Bqf = QF[i2]; qbk, Bqbk = QBk[i2]
                    mm(po[:, 0:256], sm[:], vv[:, c, :], True, False, [Bsm, Bvv], [Bpo])
                    mm(po[:, 0:256], qf[:], Sf16[:, c, :], False, False, [Bqf, BSf16], [Bpo])
                    mm(po[:, 0:256], qbk[:], Sb16[:, c, :], False, True, [Bqbk, BSb16], [Bpo])
                    st_, Bst = STT[i2]; mv, Bmv = MV[i2]; rs, Brs = RS[i2]; ot, Bot = OT[i2]; r16, Br16 = R16[i2]
                    S.op("dve", lambda e: e.bn_stats(st_[:], po[:, 0:256]), r=[Bpo], w=[Bst])
                    S.op("dve", lambda e: e.bn_aggr(mv[:], st_[:]), r=[Bst], w=[Bmv])
                    S.op("act", lambda e: e.activation(rs[:], mv[:, 1:2], AF.Sqrt, bias=epsb[:, 0:1]),
                         r=[Bmv, Bepsb], w=[Brs])
                    S.op("dve", lambda e: e.scalar_tensor_tensor(
                        out=ot[:], in0=po[:, 0:256], scalar=mv[:, 0:1], in1=sg[:, c, :], op0=ALU.subtract, op1=ALU.mult),
                        r=[Bpo, Bmv, Bsg], w=[Bot])
                    S.op("dve", lambda e: e.reciprocal(rs[:], rs[:]), r=[Brs], w=[Brs])
                    S.op("act", lambda e: e.activation(r16[:], ot[:], AF.Copy, scale=rs[:, 0:1]),
                         r=[Bot, Brs], w=[Br16])

                def sw_c(c):
                    cs = slice(c * 128, (c + 1) * 128)
                    i2 = c % 2
                    r16, Br16 = R16[i2]
                    pb_, Bpb_ = PSBs[i2]
                    for vt in range(2):
                        S.op("pe", lambda e, vt=vt: e.transpose(
                            pb_[:, vt * 128:(vt + 1) * 128], r16[:, vt * 128:(vt + 1) * 128], identb),
                            r=[Br16, Bcstb], w=[Bpb_])
                    S.op("act", lambda e: e.activation(
                        retT[:, :, cs], pb_[:, 0:256].rearrange("p (a b) -> p a b", a=2), AF.Copy),
                        r=[Bpb_], w=[BretT])

                for s_ in range(NT + 2):
                    if s_ < NT:
                        sw_a(s_)
                    if 1 <= s_ <= NT:
                        sw_b(s_ - 1)
                    if s_ >= 2:
                        sw_c(s_ - 2)
                S.dma("act", retT_d[:, 2 * h:2 * h + 2, :], retT[:], BretT, r=[BretT], w=[dbuf_ret])
                if stage == "p2" and h in (0, 7):
                    with scope() as pd:
                        tmpf, Btmpf = sb("tmpf%d" % h, [128, 1024], stack=pd)
                        for a in range(2):
                            for q4 in range(2):
                                S.op("dve", lambda e, a=a, q4=q4: e.tensor_copy(tmpf[:], retT[:, a, q4 * 1024:(q4 + 1) * 1024]),
                                     r=[BretT], w=[Btmpf])
                                dbg_dump(tmpf[:], Btmpf, 128, 1024, (0 if h == 0 else 4096) + a * 2048 + q4 * 1024)
                        S.wait_all("sp", [Btmpf])
            if stage == "p2":
                print("counts", S.cnt, S.nd)
                S.wait_all("sp", [dbuf, BretT])
                return nc

        p12cm.__exit__(None, None, None)

        def stopat(tag):
            if os.environ.get("STOPAT") == tag:
                S.wait_all("sp", list(all_bufs))
                return True
            return False

        Ycm = scope(); Yst = Ycm.__enter__()
        comb, Bcomb = sb("comb", [128, 8, T], BF16, stack=Yst)
        Zcm = scope(); Zst = Zcm.__enter__()
        z2, Bz2 = sb("z2", [128, 8, T], BF16, stack=Zst)
        ZW = T + 32
        with scope() as p3a:
            zext, Bzext = sb("zext", [128, 8, ZW], BF16, stack=p3a)
            WZ = [sb("wz%d" % i, [128, 8, 256], BF16, stack=p3a) for i in range(2)]
            SIG = [sb("sig%d" % i, [128, 512], stack=p3a) for i in range(2)]
            zh, Bzh = sb("zh", [128, 32], stack=p3a)
            DG = [sb("dg%d" % i, [128, 31, 128], BF16, stack=p3a) for i in range(2)]
            it = 0
            for ct in range(8):
                wz, Bwz = WZ[ct % 2]
                c_a = GLU_OFF + ct * 128
                c_b = GLU_OFF + 1024 + ct * 128
                S.dma("pool", wz[:, :, 0:128], w_in[:, c_a:c_a + 128].rearrange("(k p) c -> p k c", p=128), Bwz, w=[Bwz])
                S.dma("pool", wz[:, :, 128:256], w_in[:, c_b:c_b + 128].rearrange("(k p) c -> p k c", p=128), Bwz,
                      w=[Bwz], join=True)
                bza = fm[:, R_BIN + 48 + ct:R_BIN + 49 + ct]
                bzb = fm[:, R_BIN + 56 + ct:R_BIN + 57 + ct]
                segs = [("own", blk) for blk in range(4)] + [("halo", 0)]
                for kind, blk in segs:
                    n = 512 if kind == "own" else 32
                    pa, Bpa = PS[(it % 3) * 2]
                    pb, Bpb = PS[(it % 3) * 2 + 1]
                    sig, Bsig = SIG[it % 2]
                    it += 1
                    for k in range(8):
                        rhs = u_own[:, k, blk * 512:(blk + 1) * 512] if kind == "own" else u_halo[:, k, :]
                        mm(pa[:, 0:n], wz[:, k, 0:128], rhs, k == 0, k == 7, [Bwz, Bu, Buh], [Bpa])
                    for k in range(8):
                        rhs = u_own[:, k, blk * 512:(blk + 1) * 512] if kind == "own" else u_halo[:, k, :]
                        mm(pb[:, 0:n], wz[:, k, 128:256], rhs, k == 0, k == 7, [Bwz, Bu, Buh], [Bpb])
                    S.op("act", lambda e, sig=sig, pb=pb, n=n, bzb=bzb: e.activation(sig[:, 0:n], pb[:, 0:n], AF.Sigmoid, bias=bzb),
                         r=[Bpb, Bfm], w=[Bsig])
                    if kind == "own":
                        S.op("dve", lambda e, pa=pa, sig=sig, bza=bza, ct=ct, blk=blk: e.scalar_tensor_tensor(
                            out=zext[:, ct, 16 + blk * 512:16 + (blk + 1) * 512], in0=pa[:, 0:512], scalar=bza,
                            in1=sig[:, 0:512], op0=ALU.add, op1=ALU.mult), r=[Bpa, Bsig, Bfm], w=[Bzext])
                    else:
                        S.op("dve", lambda e, pa=pa, sig=sig, bza=bza: e.scalar_tensor_tensor(
                            out=zh[:], in0=pa[:, 0:32], scalar=bza, in1=sig[:, 0:32], op0=ALU.add, op1=ALU.mult),
                            r=[Bpa, Bsig, Bfm], w=[Bzh])
                        S.op("dve", lambda e, ct=ct: e.tensor_tensor(zext[:, ct, 0:16], zh[:, 0:16], cst[:, C_HM:C_HM + 16], ALU.mult),
                             r=[Bzh, Bcst], w=[Bzext])
                        S.op("dve", lambda e, ct=ct: e.tensor_tensor(zext[:, ct, ZW - 16:ZW], zh[:, 16:32],
                                                                  cst[:, C_HM + 16:C_HM + 32], ALU.mult),
                             r=[Bzh, Bcst], w=[Bzext])
            def build_diag(ct):
                dg, Bdg = DG[ct % 2]
                for t in range(31):
                    col = R_CW + t * 8 + ct
                    if t % 2 == 0:
                        S.op("dve", lambda e, dg=dg, t=t, col=col: e.tensor_scalar_mul(dg[:, t, :], identb, fm[:, col:col + 1]),
                             r=[Bcstb, Bfm], w=[Bdg])
                    else:
                        S.op("act", lambda e, dg=dg, t=t, col=col: e.activation(dg[:, t, :], identb, AF.Copy, scale=fm[:, col:col + 1]),
                             r=[Bcstb, Bfm], w=[Bdg])

            it = 0
            build_diag(0)
            for ct in range(8):
                dg, Bdg = DG[ct % 2]
                if ct + 1 < 8:
                    build_diag(ct + 1)
                for blk in range(4):
                    pc, Bpc = PS[it % 6]
                    it += 1
                    for t in range(31):
                        o0 = blk * 512 + t + 1
                        mm(pc[:], dg[:, t, :], zext[:, ct, o0:o0 + 512], t == 0, t == 30, [Bdg, Bzext], [Bpc])
                    S.op("act", lambda e, pc=pc, ct=ct, blk=blk: e.activation(
                        z2[:, ct, blk * 512:(blk + 1) * 512], pc[:], AF.Identity, bias=fm[:, R_CB + ct:R_CB + ct + 1]),
                        r=[Bpc, Bfm], w=[Bz2])
        if stage == "p3a":
            with scope() as pd:
                tmpf, Btmpf = sb("tmpfz", [128, 2048], stack=pd)
                for ct in (0, 7):
                    S.op("dve", lambda e, ct=ct: e.tensor_copy(tmpf[:], z2[:, ct, :]), r=[Bz2], w=[Btmpf])
                    dbg_dump(tmpf[:], Btmpf, 128, 2048, 0 if ct == 0 else 2048)
                S.wait_all("sp", [Btmpf])
            Zcm.__exit__(None, None, None); Ycm.__exit__(None, None, None)
            return nc
        with scope() as p3b:
            sq, Bsq = sb("sq", [128, 8, 512], BF16, stack=p3b)
            mean, Bmean = sb("mean", [128, 512], stack=p3b)
            var, Bvar = sb("var", [128, 512], stack=p3b)
            TMP = [sb("lnt%d" % i, [128, 512], stack=p3b) for i in range(2)]
            for blk in range(4):
                bs = slice(blk * 512, (blk + 1) * 512)
                S.op("act", lambda e, bs=bs: e.activation(sq[:], z2[:, :, bs], AF.Square), r=[Bz2], w=[Bsq])
                pS, BpS = PS[0]
                pQ, BpQ = PS[1]
                for ct in range(8):
                    mm(pS[:], onesb, z2[:, ct, bs], ct == 0, ct == 7, [Bcstb, Bz2], [BpS])
                for ct in range(8):
                    mm(pQ[:], onesb, sq[:, ct, :], ct == 0, ct == 7, [Bcstb, Bsq], [BpQ])
                S.op("act", lambda e: e.activation(mean[:], pS[:], AF.Copy, scale=1.0 / D), r=[BpS], w=[Bmean])
                S.op("dve", lambda e: e.tensor_tensor(var[:], mean[:], mean[:], ALU.mult), r=[Bmean], w=[Bvar])
                S.op("dve", lambda e: e.scalar_tensor_tensor(out=var[:], in0=pQ[:], scalar=1.0 / D, in1=var[:],
                                                             op0=ALU.mult, op1=ALU.subtract), r=[BpQ, Bvar], w=[Bvar])
                S.op("act", lambda e: e.activation(var[:], var[:], AF.Sqrt, bias=epsb[:, 0:1]), r=[Bvar, Bepsb], w=[Bvar])
                S.op("dve", lambda e: e.reciprocal(var[:], var[:]), r=[Bvar], w=[Bvar])
                for ct in range(8):
                    tm, Btm = TMP[ct % 2]
                    S.op("dve", lambda e, tm=tm, ct=ct, bs=bs: e.tensor_tensor(tm[:], z2[:, ct, bs], mean[:], ALU.subtract),
                         r=[Bz2, Bmean], w=[Btm])
                    S.op("dve", lambda e, tm=tm: e.tensor_tensor(tm[:], tm[:], var[:], ALU.mult), r=[Btm, Bvar], w=[Btm])
                    S.op("act", lambda e, tm=tm, ct=ct, bs=bs: e.activation(
                        z2[:, ct, bs], tm[:], AF.Silu, bias=fm[:, R_LB + ct:R_LB + ct + 1], scale=fm[:, R_LG + ct:R_LG + ct + 1]),
                        r=[Btm, Bfm], w=[Bz2])
        if stage == "p3b":
            with scope() as pd:
                tmpf, Btmpf = sb("tmpfz", [128, 2048], stack=pd)
                for ct in (0, 7):
                    S.op("dve", lambda e, ct=ct: e.tensor_copy(tmpf[:], z2[:, ct, :]), r=[Bz2], w=[Btmpf])
                    dbg_dump(tmpf[:], Btmpf, 128, 2048, 0 if ct == 0 else 2048)
                S.wait_all("sp", [Btmpf])
            Zcm.__exit__(None, None, None); Ycm.__exit__(None, None, None)
            return nc

        with scope() as p3c:
            RB = [sb("rb%d" % i, [128, 16, 512], BF16, stack=p3c) for i in range(2)]
            WJ = [dict(wro=sb("wro%d" % i, [128, 2, 16, 128], BF16, stack=p3c), wco=sb("wco%d" % i, [128, 2, 8, 128], BF16, stack=p3c),
                       wg=sb("wgj%d" % i, [128, 2, 8, 256], BF16, stack=p3c)) for i in range(2)]
            GA = [sb("ga%d" % i, [128, 512], stack=p3c) for i in range(2)]
            GB = [sb("gb%d" % i, [128, 512], stack=p3c) for i in range(2)]
            TA = [sb("ta%d" % i, [128, 512], stack=p3c) for i in range(2)]
            TB = [sb("tb%d" % i, [128, 512], stack=p3c) for i in range(2)]
            it = 0
            ib = 0
            for jp in range(4):
                wro, Bwro = WJ[jp % 2]["wro"]; wco, Bwco = WJ[jp % 2]["wco"]; wgj, Bwgj = WJ[jp % 2]["wg"]
                for jj in range(2):
                    j = jp * 2 + jj
                    js = slice(j * 128, (j + 1) * 128)
                    S.dma("pool", wro[:, jj, :, :], w_ro[:, js].rearrange("(k p) c -> p k c", p=128), Bwro, w=[Bwro], join=(jj > 0))
                    S.dma("pool", wco[:, jj, :, :], w_co[:, js].rearrange("(k p) c -> p k c", p=128), Bwco, w=[Bwco], join=(jj > 0))
                    S.dma("pool", wgj[:, jj, :, 0:128], w_in[:, GATE_OFF + j * 128:GATE_OFF + (j + 1) * 128].rearrange(
                        "(k p) c -> p k c", p=128), Bwgj, w=[Bwgj], join=(jj > 0))
                    S.dma("pool", wgj[:, jj, :, 128:256], w_in[:, GATE_OFF + 1024 + j * 128:GATE_OFF + 1024 + (j + 1) * 128].rearrange(
                        "(k p) c -> p k c", p=128), Bwgj, w=[Bwgj], join=True)
                for blk in range(4):
                    bs = slice(blk * 512, (blk + 1) * 512)
                    rb, Brb = RB[ib % 2]
                    ib += 1
                    S.dma("sp", rb[:], retT_d[:, :, bs], Brb, r=[dbuf_ret], w=[Brb])
                    for jj in range(2):
                        j = jp * 2 + jj
                        i2 = it % 2
                        it += 1
                        pga, Bpga = PS[4]
                        pgb, Bpgb = PS[5]
                        pya, Bpya = PS[i2 * 2]
                        pyb, Bpyb = PS[i2 * 2 + 1]
                        ga, Bga = GA[i2]; gb, Bgb = GB[i2]; ta, Bta = TA[i2]; tb, Btb = TB[i2]
                        for k in range(8):
                            mm(pga[:], wgj[:, jj, k, 0:128], u_own[:, k, bs], k == 0, k == 7, [Bwgj, Bu], [Bpga])
                        for k in range(8):
                            mm(pgb[:], wgj[:, jj, k, 128:256], u_own[:, k, bs], k == 0, k == 7, [Bwgj, Bu], [Bpgb])
                        S.op("act", lambda e, ga=ga, pga=pga, j=j: e.activation(
                            ga[:], pga[:], AF.Sigmoid, bias=fm[:, R_BIN + 64 + j:R_BIN + 65 + j]), r=[Bpga, Bfm], w=[Bga])
                        S.op("act", lambda e, gb=gb, pgb=pgb, j=j: e.activation(
                            gb[:], pgb[:], AF.Sigmoid, bias=fm[:, R_BIN + 72 + j:R_BIN + 73 + j]), r=[Bpgb, Bfm], w=[Bgb])
                        for k in range(16):
                            mm(pya[:], wro[:, jj, k, :], rb[:, k, :], k == 0, k == 15, [Bwro, Brb], [Bpya])
                        for k in range(8):
                            mm(pyb[:], wco[:, jj, k, :], z2[:, k, bs], k == 0, k == 7, [Bwco, Bz2], [Bpyb])
                        S.op("dve", lambda e, ta=ta, pya=pya, ga=ga: e.tensor_tensor(ta[:], pya[:], ga[:], ALU.mult),
                             r=[Bpya, Bga], w=[Bta])
                        S.op("dve", lambda e, tb=tb, pyb=pyb, gb=gb, j=j: e.scalar_tensor_tensor(
                            out=tb[:], in0=pyb[:], scalar=fm[:, R_BCO + j:R_BCO + j + 1], in1=gb[:], op0=ALU.add, op1=ALU.mult),
                            r=[Bpyb, Bgb, Bfm], w=[Btb])
                        S.op("dve", lambda e, ta=ta, tb=tb, j=j, bs=bs: e.tensor_tensor(comb[:, j, bs], ta[:], tb[:], ALU.add),
                             r=[Bta, Btb], w=[Bcomb])
        Zcm.__exit__(None, None, None)
        if stage == "p3c":
            with scope() as pd:
                tmpf, Btmpf = sb("tmpfz", [128, 2048], stack=pd)
                for ct in (0, 7):
                    S.op("dve", lambda e, ct=ct: e.tensor_copy(tmpf[:], comb[:, ct, :]), r=[Bcomb], w=[Btmpf])
                    dbg_dump(tmpf[:], Btmpf, 128, 2048, 0 if ct == 0 else 2048)
                S.wait_all("sp", [Btmpf])
            Ycm.__exit__(None, None, None)
            return nc

        tTb, BtTb = u_own, Bu
        BIG = 1.0e4
        with scope() as p3d:
            wmo, Bwmo = sb("wmo", [128, 8, D], BF16, stack=p3d)
            S.dma("pool", wmo[:], w_mo.rearrange("(k p) c -> p k c", p=128), Bwmo, w=[Bwmo])
            rows = {}
            for nm, src in (("bmix", b_mo), ("l1g", ln1g), ("l1b", ln1b)):
                rows[nm] = sb(nm, [128, D], stack=p3d)
                S.dma("sp", rows[nm][0][:], src.partition_broadcast(128), rows[nm][1], w=[rows[nm][1]])
            wr32, Bwr32 = sb("wr32", [128, 8, 36], stack=p3d)
            S.dma("sp", wr32[:], w_rt.rearrange("(k p) c -> p k c", p=128), Bwr32, w=[Bwr32])
            brt, Bbrt = sb("brt", [128, 36], stack=p3d)
            S.dma("sp", brt[:], b_rt.partition_broadcast(128), Bbrt, w=[Bbrt])
            XT = [sb("x3_%d" % i, [128, D], stack=p3d) for i in range(2)]
            HP = [sb("hp%d" % i, [128, D], stack=p3d) for i in range(2)]
            TT32 = [sb("tt32_%d" % i, [128, 8, 128], stack=p3d) for i in range(2)]
            ST = [sb("st3_%d" % i, [128, 12], stack=p3d) for i in range(2)]
            MV3 = [sb("mv3_%d" % i, [128, 2], stack=p3d) for i in range(2)]
            RS3 = [sb("rs3_%d" % i, [128, 1], stack=p3d) for i in range(2)]
            Lall, BLall = sb("Lall", [128, NT, 36], stack=p3d)
            bmix, Bbmix = rows["bmix"]; l1g, Bl1g = rows["l1g"]; l1b, Bl1b = rows["l1b"]
            bmb, Bbmb = sb("bmb", [1, D], BF16, stack=p3d)
            S.dma("pool", bmb[:], b_mo, Bbmb, w=[Bbmb])
            gs8, Bgs8 = sb("gs8", [128, 8], stack=p3d)
            bs8, Bbs8 = sb("bs8", [128, 8], stack=p3d)
            S.op("dve", lambda e: e.tensor_tensor(gs8[:], fm[:, R_L1G:R_L1G + 8], sc1f[:], ALU.mult), r=[Bfm, Bsc1f], w=[Bgs8])
            S.op("dve", lambda e: e.tensor_tensor(bs8[:], fm[:, R_L1B:R_L1B + 8], sc1f[:], ALU.mult), r=[Bfm, Bsc1f], w=[Bbs8])
            S.op("dve", lambda e: e.tensor_tensor(bs8[:], bs8[:], modB[:, 0:8, 0], ALU.add), r=[Bbs8, BmodB], w=[Bbs8])
            NM = [sb("nmr%d" % i, [128, 1], stack=p3d) for i in range(2)]
            HN = [sb("hn%d" % i, [128, D], stack=p3d) for i in range(2)]

            def ln_norm(hp, Bhp, hn, Bhn, st_, Bst, mv, Bmv, rs, Brs, nm, Bnm):
                S.op("dve", lambda e: e.bn_stats(st_[:, 0:6], hp[:, 0:512]), r=[Bhp], w=[Bst])
                S.op("dve", lambda e: e.bn_stats(st_[:, 6:12], hp[:, 512:1024]), r=[Bhp], w=[Bst])
                S.op("dve", lambda e: e.bn_aggr(mv[:], st_[:]), r=[Bst], w=[Bmv])
                S.op("act", lambda e: e.activation(rs[:], mv[:, 1:2], AF.Ln, bias=epsb[:, 0:1]), r=[Bmv, Bepsb], w=[Brs])
                S.op("act", lambda e: e.activation(rs[:], rs[:], AF.Exp, scale=-0.5), r=[Brs], w=[Brs])
                S.op("dve", lambda e: e.scalar_tensor_tensor(out=nm[:], in0=mv[:, 0:1], scalar=-1.0, in1=rs[:],
                                                             op0=ALU.mult, op1=ALU.mult), r=[Bmv, Brs], w=[Bnm])
                S.op("act", lambda e: e.activation(hn[:], hp[:], AF.Identity, bias=nm[:, 0:1], scale=rs[:, 0:1]),
                     r=[Bhp, Bnm, Brs], w=[Bhn])

            def d_s1(tile):
                i2 = tile % 2
                ts_ = slice(tile * 128, (tile + 1) * 128)
                xt3, Bxt3 = XT[i2]; hp, Bhp = HP[i2]; hn, Bhn = HN[i2]
                S.dma("sp", xt3[:], xo[ts_, :], Bxt3, w=[Bxt3])
                for half in range(2):
                    pm, Bpm = PS[half]
                    hs = slice(half * 512, (half + 1) * 512)
                    for k in range(8):
                        mm(pm[:], comb[:, k, ts_], wmo[:, k, hs], k == 0, False, [Bcomb, Bwmo], [Bpm])
                    mm(pm[:], cstb[0:1, CB_ON:CB_ON + 128], bmb[0:1, hs], False, True, [Bcstb, Bbmb], [Bpm])
                    S.op("dve", lambda e, pm=pm, hs=hs: e.tensor_tensor(hp[:, hs], pm[:], gtrow[:, 0, hs], ALU.mult),
                         r=[Bpm, Bgtrow], w=[Bhp])
                S.op("dve", lambda e: e.scalar_tensor_tensor(out=hp[:], in0=xt3[:], scalar=ALPHA, in1=hp[:],
                                                             op0=ALU.mult, op1=ALU.add), r=[Bhp, Bxt3], w=[Bhp])

            Bst1 = [Buf("h1st0"), Buf("h1st1")]

            def d_s1b(tile):
                i2 = tile % 2
                ts_ = slice(tile * 128, (tile + 1) * 128)
                hp, Bhp = HP[i2]; hn, Bhn = HN[i2]
                st_, Bst = ST[i2]; mv, Bmv = MV3[i2]; rs, Brs = RS3[i2]; nm, Bnm = NM[i2]
                ln_norm(hp, Bhp, hn, Bhn, st_, Bst, mv, Bmv, rs, Brs, nm, Bnm)
                S.op("dve", lambda e: e.tensor_tensor(hp[:], hn[:], l1g[:], ALU.mult), r=[Bhn, Bl1g], w=[Bhp])
                S.op("dve", lambda e: e.tensor_tensor(hp[:], hp[:], l1b[:], ALU.add), r=[Bhp, Bl1b], w=[Bhp])
                S.dma("pool", h1_d[ts_, :], hp[:], Bst1[i2], r=[Bhp], w=[dbuf_h1])

            def d_s2(tile):
                i2 = tile % 2
                ts_ = slice(tile * 128, (tile + 1) * 128)
                hn, Bhn = HN[i2]; t32, Bt32 = TT32[i2]; hp, Bhp = HP[i2]
                pa, Bpa = PS[2]
                pb, Bpb = PS[3]
                pr_, Bpr_ = PS[4 + i2]
                for dt in range(8):
                    pp, Bpp = (pa, Bpa) if dt < 4 else (pb, Bpb)
                    S.op("pe", lambda e, dt=dt, pp=pp: e.transpose(
                        pp[:, (dt % 4) * 128:(dt % 4 + 1) * 128], hn[:, dt * 128:(dt + 1) * 128], ident), r=[Bhn, Bcst], w=[Bpp])
                for dt in range(8):
                    pp, Bpp = (pa, Bpa) if dt < 4 else (pb, Bpb)
                    S.op("act", lambda e, dt=dt, pp=pp: e.activation(
                        t32[:, dt, :], pp[:, (dt % 4) * 128:(dt % 4 + 1) * 128], AF.Identity,
                        bias=bs8[:, dt:dt + 1], scale=gs8[:, dt:dt + 1]), r=[Bpp, Bbs8, Bgs8], w=[Bt32])
                S.op("dve", lambda e: e.tensor_copy(tTb[:, :, ts_], t32[:]), r=[Bt32], w=[BtTb])
                for dt in range(8):
                    mm(pr_[:, 0:36], t32[:, dt, :], wr32[:, dt, :], dt == 0, dt == 7, [Bt32, Bwr32], [Bpr_])
                S.op("dve", lambda e: e.tensor_tensor(Lall[:, tile, :], pr_[:, 0:36], brt[:], ALU.add), r=[Bpr_, Bbrt], w=[BLall])
                if stage == "p3d" and tile in (0, 15):
                    dbg_dump(hp[:], Bhp, 128, 1024, 0 if tile == 0 else 1024)

            d_s1(0)
            d_s1b(0)
            for tile in range(NT):
                if tile + 1 < NT:
                    d_s1(tile + 1)
                d_s2(tile)
                if tile + 1 < NT:
                    d_s1b(tile + 1)

            X_ = mybir.AxisListType.X
            rt = {}
            for nm, shp in (("gmax", [128, NT]), ("eg", [128, NT, 4]), ("gw", [128, NT]), ("goh", [128, NT, 4]),
                            ("Lm", [128, NT, 32]), ("m1", [128, NT]), ("oh1", [128, NT, 32]), ("Lm2", [128, NT, 32]),
                            ("m2", [128, NT]), ("oh2", [128, NT, 32]), ("dd", [128, NT]), ("w1", [128, NT]), ("w2", [128, NT])):
                rt[nm] = sb("rt_" + nm, shp, stack=p3d)
            g = lambda nm: rt[nm][0]
            B_ = lambda nm: rt[nm][1]
            LG = Lall[:, :, 0:4]
            LE = Lall[:, :, 4:36]
            bc = lambda ap2, n: ap2.unsqueeze(2).to_broadcast([128, NT, n])
            S.op("dve", lambda e: e.tensor_reduce(g("gmax")[:], LG, X_, ALU.max), r=[BLall], w=[B_("gmax")])
            S.op("dve", lambda e: e.tensor_tensor(g("eg")[:], LG, bc(g("gmax")[:], 4), ALU.subtract), r=[BLall, B_("gmax")], w=[B_("eg")])
            S.op("act", lambda e: e.activation(g("eg")[:], g("eg")[:], AF.Exp), r=[B_("eg")], w=[B_("eg")])
            S.op("dve", lambda e: e.tensor_reduce(g("gw")[:], g("eg")[:], X_, ALU.add), r=[B_("eg")], w=[B_("gw")])
            S.op("dve", lambda e: e.reciprocal(g("gw")[:], g("gw")[:]), r=[B_("gw")], w=[B_("gw")])
            S.op("dve", lambda e: e.tensor_tensor(g("goh")[:], LG, bc(g("gmax")[:], 4), ALU.is_ge), r=[BLall, B_("gmax")], w=[B_("goh")])
            S.op("dve", lambda e: e.tensor_scalar(g("goh")[:], g("goh")[:], 1.0, BIG, ALU.subtract, ALU.mult), r=[B_("goh")], w=[B_("goh")])
            S.op("dve", lambda e: e.tensor_tensor(
                g("Lm")[:].rearrange("p t (a b) -> p t a b", a=4), LE.rearrange("p t (a b) -> p t a b", a=4),
                g("goh")[:].unsqueeze(3).to_broadcast([128, NT, 4, 8]), ALU.add), r=[BLall, B_("goh")], w=[B_("Lm")])
            S.op("dve", lambda e: e.tensor_reduce(g("m1")[:], g("Lm")[:], X_, ALU.max), r=[B_("Lm")], w=[B_("m1")])
            S.op("dve", lambda e: e.tensor_tensor(g("oh1")[:], g("Lm")[:], bc(g("m1")[:], 32), ALU.is_ge), r=[B_("Lm"), B_("m1")], w=[B_("oh1")])
            S.op("dve", lambda e: e.scalar_tensor_tensor(out=g("Lm2")[:], in0=g("oh1")[:], scalar=-BIG, in1=g("Lm")[:],
                                                         op0=ALU.mult, op1=ALU.add), r=[B_("oh1"), B_("Lm")], w=[B_("Lm2")])
            S.op("dve", lambda e: e.tensor_reduce(g("m2")[:], g("Lm2")[:], X_, ALU.max), r=[B_("Lm2")], w=[B_("m2")])
            S.op("dve", lambda e: e.tensor_tensor(g("oh2")[:], g("Lm2")[:], bc(g("m2")[:], 32), ALU.is_ge), r=[B_("Lm2"), B_("m2")], w=[B_("oh2")])
            S.op("dve", lambda e: e.tensor_tensor(g("dd")[:], g("m1")[:], g("m2")[:], ALU.subtract), r=[B_("m1"), B_("m2")], w=[B_("dd")])
            S.op("act", lambda e: e.activation(g("dd")[:], g("dd")[:], AF.Exp, scale=-1.0), r=[B_("dd")], w=[B_("dd")])
            S.op("dve", lambda e: e.tensor_scalar_add(g("dd")[:], g("dd")[:], 1.0), r=[B_("dd")], w=[B_("dd")])
            S.op("dve", lambda e: e.reciprocal(g("dd")[:], g("dd")[:]), r=[B_("dd")], w=[B_("dd")])
            S.op("dve", lambda e: e.tensor_tensor(g("w1")[:], g("dd")[:], g("gw")[:], ALU.mult), r=[B_("dd"), B_("gw")], w=[B_("w1")])
            S.op("dve", lambda e: e.tensor_tensor(g("w2")[:], g("gw")[:], g("w1")[:], ALU.subtract), r=[B_("gw"), B_("w1")], w=[B_("w2")])
            S.op("dve", lambda e: e.tensor_tensor(combw[:], g("oh1")[:], bc(g("w1")[:], 32), ALU.mult), r=[B_("oh1"), B_("w1")], w=[Bcombw])
            S.op("dve", lambda e: e.tensor_tensor(g("oh2")[:], g("oh2")[:], bc(g("w2")[:], 32), ALU.mult), r=[B_("oh2"), B_("w2")], w=[B_("oh2")])
            S.op("dve", lambda e: e.tensor_tensor(combw[:], combw[:], g("oh2")[:], ALU.add), r=[B_("oh2"), Bcombw], w=[Bcombw])
        Ycm.__exit__(None, None, None)
        if stage == "p3d":
            dbg_dump(combw[:].rearrange("p a b -> p (a b)"), Bcombw, 128, 512, 2048)
            S.wait_all("sp", list(all_bufs))
            return nc

        with scope() as p4:
            acc, Bacc = sb("acc", [128, NT, D], stack=p4)
            EW = [dict(wg=sb("ewg%d" % i, [128, 8, FF], BF16, stack=p4), wu=sb("ewu%d" % i, [128, 8, FF], BF16, stack=p4),
                       wd=sb("ewd%d" % i, [128, 4, D], BF16, stack=p4)) for i in range(2)]
            ACTT = [sb("actT%d" % i, [128, 4, 512], BF16, stack=p4) for i in range(2)]
            SGM = [sb("sgm%d" % i, [128, 512], stack=p4) for i in range(2)]
            l2g, Bl2g = sb("l2g", [128, D], stack=p4)
            l2b, Bl2b = sb("l2b", [128, D], stack=p4)
            S.dma("sp", l2g[:], ln2g.partition_broadcast(128), Bl2g, w=[Bl2g])
            S.dma("sp", l2b[:], ln2b.partition_broadcast(128), Bl2b, w=[Bl2b])
            H1 = [sb("h1_%d" % i, [128, D], stack=p4) for i in range(2)]
            ST4 = [sb("st4_%d" % i, [128, 12], stack=p4) for i in range(2)]
            MV4 = [sb("mv4_%d" % i, [128, 2], stack=p4) for i in range(2)]
            RS4 = [sb("rs4_%d" % i, [128, 1], stack=p4) for i in range(2)]
            AT = [sb("at_%d" % i, [128, D], stack=p4) for i in range(2)]
            Bost = [Buf("ost0"), Buf("ost1")]
            all_bufs.extend(Bost)
            NM4 = [sb("nm4_%d" % i, [128, 1], stack=p4) for i in range(2)]
            def final_tile(tile):
                i2 = tile % 2
                ts_ = slice(tile * 128, (tile + 1) * 128)
                h1t, Bh1t = H1[i2]
                at_, Bat = AT[i2]
                S.dma("sp", h1t[:], h1_d[ts_, :], Bh1t, r=[dbuf_h1], w=[Bh1t])
                S.op("dve", lambda e, tile=tile, at_=at_: e.tensor_tensor(at_[:], acc[:, tile, :], gtrow[:, 1, :], ALU.mult),
                     r=[Bacc, Bgtrow], w=[Bat])
                S.op("dve", lambda e, h1t=h1t, at_=at_: e.scalar_tensor_tensor(
                    out=at_[:], in0=h1t[:], scalar=ALPHA, in1=at_[:], op0=ALU.mult, op1=ALU.add),
                    r=[Bh1t, Bat], w=[Bat])
                st_, Bst = ST4[i2]; mv, Bmv = MV4[i2]; rs, Brs = RS4[i2]; nm, Bnm = NM4[i2]
                ln_norm(at_, Bat, h1t, Bh1t, st_, Bst, mv, Bmv, rs, Brs, nm, Bnm)
                S.op("dve", lambda e, h1t=h1t: e.tensor_tensor(h1t[:], h1t[:], l2g[:], ALU.mult), r=[Bh1t, Bl2g], w=[Bh1t])
                S.op("pool", lambda e, h1t=h1t: e.tensor_tensor(h1t[:], h1t[:], l2b[:], ALU.add), r=[Bh1t, Bl2b], w=[Bh1t])
                S.dma("pool", out_d[ts_, :], h1t[:], Bost[i2], r=[Bh1t])
            NEX = int(os.environ.get("NEXP", NE))
            it = 0
            iyc = [0]
            pend_dn = []
            for ex in range(NEX):
                ewg, Bewg = EW[ex % 2]["wg"]; ewu, Bewu = EW[ex % 2]["wu"]; ewd, Bewd = EW[ex % 2]["wd"]
                S.dma("pool", ewg[:], w_eg[ex].rearrange("(k p) c -> p k c", p=128), Bewg, w=[Bewg])
                S.dma("pool", ewu[:], w_eu[ex].rearrange("(k p) c -> p k c", p=128), Bewu, w=[Bewu])
                S.dma("pool", ewd[:], w_ed[ex].rearrange("(k p) c -> p k c", p=128), Bewd, w=[Bewd])
                for blk in range(4):
                    bs = slice(blk * 512, (blk + 1) * 512)
                    actT, BactT = ACTT[blk % 2]
                    for fc in range(4):
                        pg, Bpg = PS[(it % 2) * 2]
                        pu, Bpu = PS[(it % 2) * 2 + 1]
                        sgm, Bsgm = SGM[it % 2]
                        it += 1
                        for k in range(8):
                            mm(pg[:], ewg[:, k, fc * 128:(fc + 1) * 128], tTb[:, k, bs], k == 0, k == 7, [Bewg, BtTb], [Bpg])
                        for k in range(8):
                            mm(pu[:], ewu[:, k, fc * 128:(fc + 1) * 128], tTb[:, k, bs], k == 0, k == 7, [Bewu, BtTb], [Bpu])
                        S.op("act", lambda e, sgm=sgm, pg=pg: e.activation(sgm[:], pg[:], AF.Silu), r=[Bpg], w=[Bsgm])
                        S.op("dve", lambda e, actT=actT, fc=fc, sgm=sgm, pu=pu: e.tensor_tensor(actT[:, fc, :], sgm[:], pu[:], ALU.mult),
                             r=[Bsgm, Bpu], w=[BactT])

                    def down(ex=ex, blk=blk, actT=actT, BactT=BactT, ewd=ewd, Bewd=Bewd):
                        for tl in range(4):
                            tile = blk * 4 + tl
                            for half in range(2):
                                py, Bpy = PS[4 + iyc[0] % 2]
                                iyc[0] += 1
                                for fc in range(4):
                                    mm(py[:], actT[:, fc, tl * 128:(tl + 1) * 128], ewd[:, fc, half * 512:(half + 1) * 512],
                                       fc == 0, fc == 3, [BactT, Bewd], [Bpy])
                                a_ap = acc[:, tile, half * 512:(half + 1) * 512]
                                w_ap = combw[:, tile, ex:ex + 1]
                                if ex == 0:
                                    S.op("dve", lambda e, a_ap=a_ap, py=py, w_ap=w_ap: e.tensor_scalar(a_ap, py[:], w_ap, None, ALU.mult),
                                         r=[Bpy, Bcombw], w=[Bacc])
                                else:
                                    S.op("dve", lambda e, a_ap=a_ap, py=py, w_ap=w_ap: e.scalar_tensor_tensor(
                                        out=a_ap, in0=py[:], scalar=w_ap, in1=a_ap, op0=ALU.mult, op1=ALU.add),
                                        r=[Bpy, Bcombw, Bacc], w=[Bacc])
                            if ex == NEX - 1:
                                final_tile(tile)
                    if pend_dn:
                        pend_dn.pop(0)()
                    pend_dn.append(down)
            while pend_dn:
                pend_dn.pop(0)()
            S.wait_all("sp", list(all_bufs))
        return nc
    return nc


def _consts():
    p = np.arange(128, dtype=np.float32)
    j = p[:, None]
    i = p[None, :]
    cst = np.zeros((128, CW), np.float32)
    cst[:, C_ID:C_ID + 128] = np.eye(128, dtype=np.float32)
    cst[:, C_DF:C_DF + 128] = np.maximum(i - j, 0)
    cst[:, C_MF:C_MF + 128] = (i >= j)
    cst[:, C_DB:C_DB + 128] = np.maximum(j - i, 0)
    cst[:, C_MB:C_MB + 128] = (j >= i)
    cst[:, C_I1:C_I1 + 128] = i + 1
    cst[:, C_I2:C_I2 + 128] = 128 - i
    cst[:, C_PC] = 127 - p
    cst[:, C_PC + 1] = p
    cst[:, C_IE] = -(np.arange(128) % 32) / 32.0
    cb = np.zeros((128, CBW), np.float32)
    cb[:, CB_ID:CB_ID + 128] = np.eye(128)
    pr = np.zeros((128, 128), np.float32)
    for m in range(128):
        if m < 64:
            pr[m + 64, m] = -1.0
        else:
            pr[m - 64, m] = 1.0
    cb[:, CB_PR:CB_PR + 128] = pr
    cb[:, CB_ON:CB_ON + 128] = 1.0
    return cst, cb.astype(ml_dtypes.bfloat16)


def _angpos(tok_idx):
    tok_idx = np.asarray(tok_idx)
    row = (tok_idx // 64).astype(np.float32)
    col = (tok_idx % 64).astype(np.float32)
    sel_col = ((np.arange(128) % 64) >= 32)
    return np.where(sel_col[:, None], col[None, :], row[None, :]).astype(np.float32)


def prep_inputs(inp):
    f = lambda a: np.ascontiguousarray(np.asarray(a, dtype=np.float32))
    x = f(inp["x"]); ctx = f(inp["ctx"]); c = f(inp["c"]); c_ctx = f(inp["c_ctx"])
    cst0, cstb = _consts()
    b_in = f(inp["b_in"])[0]
    vecs = np.zeros((NVEC, 128), np.float32)
    vecs[R_BADA:R_BADA + 48] = f(inp["b_ada"])[0].reshape(48, 128)
    vecs[R_BIN:R_BIN + 80] = b_in.reshape(80, 128)
    vecs[R_CW:R_CW + 248] = f(inp["conv_dw"])[0].reshape(31 * 8, 128)
    vecs[R_CB:R_CB + 8] = f(inp["conv_dw_b"])[0].reshape(8, 128)
    vecs[R_LG:R_LG + 8] = f(inp["conv_ln_g"])[0].reshape(8, 128)
    vecs[R_LB:R_LB + 8] = f(inp["conv_ln_b"])[0].reshape(8, 128)
    vecs[R_BCO:R_BCO + 8] = f(inp["b_conv_out"])[0].reshape(8, 128)
    vecs[R_L1G:R_L1G + 8] = f(inp["ln1_g"])[0].reshape(8, 128)
    vecs[R_L1B:R_L1B + 8] = f(inp["ln1_b"])[0].reshape(8, 128)
    shared = {
        "cstb": cstb, "vecs": vecs,
        "w_ada": f(inp["w_ada"])[0], "b_ada": f(inp["b_ada"]), "w_in": f(inp["w_in"])[0], "b_in": f(inp["b_in"]),
        "decay": np.concatenate([f(inp["ret_decay_fwd"]), f(inp["ret_decay_bwd"])], axis=1),
        "w_ret_out": f(inp["w_ret_out"])[0], "w_conv_out": f(inp["w_conv_out"])[0],
        "w_mix_out": f(inp["w_mix_out"])[0], "b_mix_out": f(inp["b_mix_out"]),
        "ln1_g": f(inp["ln1_g"]), "ln1_b": f(inp["ln1_b"]), "ln2_g": f(inp["ln2_g"]), "ln2_b": f(inp["ln2_b"]),
        "w_router": np.ascontiguousarray(np.concatenate([f(inp["w_router_grp"])[0], f(inp["w_router_exp"])[0]], axis=1)),
        "b_router": np.concatenate([f(inp["b_router_grp"]), f(inp["b_router_exp"])], axis=1),
        "w_exp_gate": f(inp["w_exp_gate"])[0], "w_exp_up": f(inp["w_exp_up"])[0], "w_exp_down": f(inp["w_exp_down"])[0],
    }
    maps = []
    for core in range(8):
        b, s = core // 2, core % 2
        own0 = s * T
        oth0 = (1 - s) * T
        m = dict(shared)
        m["xo"] = np.ascontiguousarray(x[b, own0:own0 + T])
        m["xa"] = np.ascontiguousarray(np.concatenate([x[b, oth0:oth0 + T], ctx[b]], axis=0))
        halo = np.zeros((32, D), np.float32)
        hm = np.zeros(32, np.float32)
        if s == 1:
            halo[0:16] = x[b, own0 - 16:own0]; hm[0:16] = 1.0
        else:
            halo[16:32] = x[b, own0 + T:own0 + T + 16]; hm[16:32] = 1.0
        m["xh"] = halo
        m["cvec"] = np.ascontiguousarray(np.stack([c[b], c_ctx], axis=1))
        cst = cst0.copy()
        cst[:, C_HM:C_HM + 32] = hm[None, :]
        we = np.zeros((128, NTA, 4), np.float32)
        mo = np.arange(T, dtype=np.float32).reshape(16, 128).T
        mc = np.arange(256, dtype=np.float32).reshape(2, 128).T
        if s == 0:
            we[:, 0:16, 2] = mo; we[:, 0:16, 3] = 1.0
            we[:, 16:18, 0] = 255 - mc; we[:, 16:18, 1] = 1.0
            we[:, 16:18, 2] = mc + T; we[:, 16:18, 3] = 1.0
        else:
            we[:, 0:16, 0] = (T - 1) - mo; we[:, 0:16, 1] = 1.0
            we[:, 16:18, 0] = 255 - mc + T; we[:, 16:18, 1] = 1.0
            we[:, 16:18, 2] = mc; we[:, 16:18, 3] = 1.0
        cst[:, C_WE:C_WE + 72] = we.reshape(128, 72)
        m["cst"] = cst
        m["ango"] = _angpos(np.arange(own0, own0 + T))
        anga = np.zeros((128, NAUX), np.float32)
        anga[:, 0:T] = _angpos(np.arange(oth0, oth0 + T))
        m["anga"] = anga
        maps.append(m)
    return maps


_NC_CACHE = {}


def kernel(**inputs):
    maps = prep_inputs(inputs)
    if "nc" not in _NC_CACHE:
        _NC_CACHE["nc"] = build_program()
    nc = _NC_CACHE["nc"]
    res = run_bass_kernel_spmd(nc, maps, core_ids=list(range(8)))
    out = np.zeros((NB, SEQ, D), np.float32)
    for core in range(8):
        b, s = core // 2, core % 2
        out[b, s * T:(s + 1) * T] = np.asarray(res.results[core]["out"], dtype=np.float32)
    return out
```
